# Optimizing a Trainium2 kernel written in Bass

```python
import math
import jax
import jax.numpy as jnp
from jax import lax
import numpy as np

D_MODEL = 1024
BATCH = 8
SEQ = 2048
DEPTH = 1
DEC_BATCH = 128
DEC_SEQ = 8
PAST_LEN = 2048
PAGE_SIZE = 128

HG_HEADS = 4
HG_DIM = 128
HG_WIDTH = HG_HEADS * HG_DIM
HG_CHUNK = 16
MB_HEADS = 4
MB_DIM = 128
MB_WIDTH = MB_HEADS * MB_DIM
MB_BLOCK = 256
MB_TOPK = 3
MB_GATHER_ROWS = 128
REL_BUCKETS = 32
REL_MAX_DIST = 128
MEM_LEN = 256
XA_HEADS = 4
XA_DIM = 128
XA_WIDTH = XA_HEADS * XA_DIM
N_GROUPS = 4
EXPERTS_PER_GROUP = 8
EXPERT_TOPK = 2
EXPERT_HIDDEN = 256
NORM_EPS = 1e-6
IN_SPLITS = (HG_WIDTH, 2 * HG_WIDTH, 3 * HG_WIDTH, 4 * HG_WIDTH,
             4 * HG_WIDTH + MB_WIDTH, 4 * HG_WIDTH + 2 * MB_WIDTH, 4 * HG_WIDTH + 3 * MB_WIDTH,
             4 * HG_WIDTH + 3 * MB_WIDTH + D_MODEL)
IN_COLS = 4 * HG_WIDTH + 3 * MB_WIDTH + 2 * D_MODEL

kernel_name = 'hgrn2_moba_gated_hmoe_decode_step'


def rmsnorm(x, g):
    xf = x.astype(jnp.float32)
    y = xf * lax.rsqrt(jnp.mean(xf * xf, axis=-1, keepdims=True) + NORM_EPS)
    return (y * g.astype(jnp.float32)).astype(x.dtype)


def hgrn2_scan(q, logf, k, v, s0):
    B, T, H, K = q.shape
    C = math.gcd(T, HG_CHUNK)
    n = T // C

    def chunks(a):
        return a.reshape(B, n, C, H, a.shape[-1]).transpose(1, 0, 3, 2, 4)

    causal = jnp.tril(jnp.ones((C, C), bool))[:, :, None]

    def step(S, inp):
        qb, fb, kb, vb = inp
        b = jnp.cumsum(fb, axis=2)
        o_inter = jnp.einsum('bhck,bhkv->bhcv', qb * jnp.exp(b), S)
        diff = b[:, :, :, None, :] - b[:, :, None, :, :]
        decay = jnp.where(causal, jnp.exp(jnp.where(causal, diff, 0.0)), 0.0)
        scores = jnp.einsum('bhtk,bhtsk,bhsk->bhts', qb, decay, kb)
        o = o_inter + jnp.einsum('bhts,bhsv->bhtv', scores, vb)
        b_last = b[:, :, -1:, :]
        S = S * jnp.exp(b_last[:, :, 0, :])[..., None] + jnp.einsum('bhck,bhcv->bhkv', kb * jnp.exp(b_last - b), vb)
        return S, o

    S, o = lax.scan(step, s0, (chunks(q), chunks(logf), chunks(k), chunks(v)))
    o = o.transpose(1, 0, 3, 2, 4).reshape(B, T, H, v.shape[-1])
    return o, S


def hgrn2_branch(hq, hf, hi, hg, lb, hg_norm, s0):
    B, T, _ = hq.shape
    f = lb + (1.0 - lb) * jax.nn.sigmoid(hf.astype(jnp.float32))
    q = jax.nn.silu(hq.astype(jnp.float32))
    heads = lambda a: a.reshape(B, T, HG_HEADS, HG_DIM)
    o, s_new = hgrn2_scan(heads(q), heads(jnp.log(f)), heads(1.0 - f),
                          heads(hi.astype(jnp.float32)), s0.astype(jnp.float32))
    o = rmsnorm(o, hg_norm.reshape(HG_HEADS, HG_DIM)).reshape(B, T, HG_WIDTH)
    return (o * jax.nn.silu(hg.astype(jnp.float32))).astype(hq.dtype), s_new


def rel_bucket(rel):
    max_exact = REL_BUCKETS // 2
    d = jnp.maximum(rel, 0)
    df = jnp.maximum(d, 1).astype(jnp.float32)
    large = max_exact + (jnp.log(df / max_exact) / math.log(REL_MAX_DIST / max_exact)
                         * (REL_BUCKETS - max_exact)).astype(jnp.int32)
    large = jnp.minimum(large, REL_BUCKETS - 1)
    return jnp.where(d < max_exact, d, large)


def query_group_size(batch, t):
    cap = max(1, MB_GATHER_ROWS // batch)
    return max(d for d in range(1, min(cap, t) + 1) if t % d == 0)


def moba_attention(q, k, v, q_pos, rel_table):
    B, T, H, D = q.shape
    L = k.shape[1]
    n_blk = -(-L // MB_BLOCK)
    pad = n_blk * MB_BLOCK - L
    kb = jnp.pad(k, ((0, 0), (0, pad), (0, 0), (0, 0))).reshape(B, n_blk, MB_BLOCK, H, D)
    vb = jnp.pad(v, ((0, 0), (0, pad), (0, 0), (0, 0))).reshape(B, n_blk, MB_BLOCK, H, D)
    k_mean = jnp.mean(kb.astype(jnp.float32), axis=2)
    own = q_pos // MB_BLOCK
    gate = jnp.einsum('bthd,bnhd->bthn', q.astype(jnp.float32), k_mean)
    past = jnp.arange(n_blk)[None, :] < own[:, None]
    gate = jnp.where(past[None, :, None, :], gate, -jnp.inf)
    _, top_idx = lax.top_k(gate, min(MB_TOPK, n_blk))
    top_idx = top_idx.astype(jnp.int32)
    sel_ok = top_idx < own[None, :, None, None]
    blk_idx = jnp.concatenate([top_idx, jnp.broadcast_to(own[None, :, None, None], (B, T, H, 1)).astype(jnp.int32)], axis=-1)
    blk_ok = jnp.concatenate([sel_ok, jnp.ones((B, T, H, 1), bool)], axis=-1)
    n_sel = blk_idx.shape[-1]
    kt = kb.transpose(0, 3, 1, 2, 4)
    vt = vb.transpose(0, 3, 1, 2, 4)
    g = query_group_size(B, T)
    n_grp = T // g

    def groups(a):
        return a.reshape((B, n_grp, g) + a.shape[2:]).swapaxes(0, 1)

    b_ix = jnp.arange(B)[:, None, None, None]
    h_ix = jnp.arange(H)[None, None, :, None]
    offs = jnp.arange(MB_BLOCK, dtype=jnp.int32)
    scale = D ** -0.5

    def attend(args):
        qg, ig, okg, pg = args
        kg = kt[b_ix, h_ix, ig]
        vg = vt[b_ix, h_ix, ig]
        rel = pg[None, :, None, None, None] - (ig[..., None] * MB_BLOCK + offs)
        mask = okg[..., None] & (rel >= 0)
        bias = rel_table[rel_bucket(rel), h_ix[..., None]].astype(jnp.float32)
        logits = jnp.einsum('bghd,bghrkd->bghrk', qg, kg).astype(jnp.float32) * scale + bias
        logits = jnp.where(mask, logits, -jnp.inf).reshape(B, g, H, n_sel * MB_BLOCK)
        p = jax.nn.softmax(logits, axis=-1).reshape(B, g, H, n_sel, MB_BLOCK)
        return jnp.einsum('bghrk,bghrkd->bghd', p.astype(vg.dtype), vg)

    out = lax.map(attend, (groups(q), groups(blk_idx), groups(blk_ok), q_pos.reshape(n_grp, g)))
    return out.swapaxes(0, 1).reshape(B, T, H, D)


def memory_attention(q, mem_k, mem_v):
    logits = jnp.einsum('bthd,bmhd->bhtm', q, mem_k).astype(jnp.float32) * XA_DIM ** -0.5
    p = jax.nn.softmax(logits, axis=-1)
    return jnp.einsum('bhtm,bmhd->bthd', p.astype(mem_v.dtype), mem_v)


def hier_moe(x, w_rg, b_rg, w_re, b_re, w_ug, w_u, w_d):
    N = x.shape[0]
    g_logits = (x @ w_rg + b_rg).astype(jnp.float32)
    grp = jnp.argmax(g_logits, axis=-1)
    g_prob = jnp.take_along_axis(jax.nn.softmax(g_logits, axis=-1), grp[:, None], axis=-1)
    e_logits = (x @ w_re + b_re).astype(jnp.float32).reshape(N, N_GROUPS, EXPERTS_PER_GROUP)
    e_logits = jnp.take_along_axis(e_logits, grp[:, None, None], axis=1)[:, 0]
    top_val, top_idx = lax.top_k(e_logits, EXPERT_TOPK)
    top_w = jax.nn.softmax(top_val, axis=-1) * g_prob
    e_w = jnp.sum(jax.nn.one_hot(top_idx, EXPERTS_PER_GROUP, dtype=jnp.float32) * top_w[..., None], axis=1)
    comb = e_w[:, None, :] * jax.nn.one_hot(grp, N_GROUPS, dtype=jnp.float32)[:, :, None]
    y = jnp.zeros((N, D_MODEL), jnp.float32)
    for gi in range(N_GROUPS):
        hdn = jax.nn.silu(jnp.einsum('nd,edf->nef', x, w_ug[gi])) * jnp.einsum('nd,edf->nef', x, w_u[gi])
        hdn = hdn * comb[:, gi, :, None].astype(hdn.dtype)
        y = y + jnp.einsum('nef,efd->nd', hdn, w_d[gi]).astype(jnp.float32)
    return y.astype(x.dtype)


def decoder_layer(x, k_past, v_past, s0, pos0, mem_k, mem_v, rel_table, lb,
                  norm_mix, w_in, hg_norm, w_branch_a, w_branch_b, w_mix_out,
                  norm_xattn, w_xq, w_xo, norm_ffn, w_rg, b_rg, w_re, b_re, w_ug, w_u, w_d):
    B, T, _ = x.shape
    h = rmsnorm(x, norm_mix) @ w_in
    hq, hf, hi, hg, mq, mk, mv, ga, gb = jnp.split(h, IN_SPLITS, axis=-1)
    oa, s_new = hgrn2_branch(hq, hf, hi, hg, lb, hg_norm, s0)
    heads = lambda a: a.reshape(B, T, MB_HEADS, MB_DIM)
    k_new, v_new = heads(mk), heads(mv)
    k_all = k_new if k_past is None else jnp.concatenate([k_past.astype(k_new.dtype), k_new], axis=1)
    v_all = v_new if v_past is None else jnp.concatenate([v_past.astype(v_new.dtype), v_new], axis=1)
    q_pos = pos0 + jnp.arange(T, dtype=jnp.int32)
    ob = moba_attention(heads(mq), k_all, v_all, q_pos, rel_table).reshape(B, T, MB_WIDTH)
    merged = jax.nn.sigmoid(ga) * (oa @ w_branch_a) + jax.nn.sigmoid(gb) * (ob @ w_branch_b)
    x = x + merged @ w_mix_out
    xq = (rmsnorm(x, norm_xattn) @ w_xq).reshape(B, T, XA_HEADS, XA_DIM)
    x = x + memory_attention(xq, mem_k, mem_v).reshape(B, T, XA_WIDTH).astype(x.dtype) @ w_xo
    xf = rmsnorm(x, norm_ffn).reshape(B * T, D_MODEL)
    x = x + hier_moe(xf, w_rg, b_rg, w_re, b_re, w_ug, w_u, w_d).reshape(B, T, D_MODEL)
    return x, k_new, v_new, s_new


def setup_inputs(seed: int = 0) -> dict:
    key = jax.random.key(seed)
    keys = jax.random.split(key, 40)
    ctr = [0]

    def nrm(shape, scale):
        k = keys[ctr[0]]
        ctr[0] += 1
        return jax.random.normal(k, shape, jnp.float32) * scale

    def gain(shape):
        return 1.0 + nrm(shape, 0.02)

    n_pages = PAST_LEN // PAGE_SIZE
    n_pool = (DEC_BATCH * n_pages * 5) // 4
    GE = N_GROUPS * EXPERTS_PER_GROUP
    x_prompt = nrm((BATCH, SEQ, D_MODEL), 1.0)
    x_sample = nrm((DEC_BATCH, DEC_SEQ, D_MODEL), 1.0)
    cache_k = nrm((DEPTH, n_pool, PAGE_SIZE, MB_HEADS, MB_DIM), 1.0)
    cache_v = nrm((DEPTH, n_pool, PAGE_SIZE, MB_HEADS, MB_DIM), 1.0)
    state_hgrn = nrm((DEPTH, DEC_BATCH, HG_HEADS, HG_DIM, HG_DIM), 0.3)
    cache_mem_k = nrm((DEPTH, DEC_BATCH, MEM_LEN, XA_HEADS, XA_DIM), 1.0)
    cache_mem_v = nrm((DEPTH, DEC_BATCH, MEM_LEN, XA_HEADS, XA_DIM), 1.0)
    perm = jax.random.permutation(keys[ctr[0]], n_pool)
    ctr[0] += 1
    page_table = perm[:DEC_BATCH * n_pages].reshape(DEC_BATCH, n_pages).astype(jnp.int32)
    mem_prompt = nrm((BATCH, MEM_LEN, D_MODEL), 1.0)
    return {
        'x_prompt': x_prompt,
        'x_sample': x_sample,
        'cache_k': cache_k,
        'cache_v': cache_v,
        'state_hgrn': state_hgrn,
        'cache_mem_k': cache_mem_k,
        'cache_mem_v': cache_mem_v,
        'page_table': page_table,
        'mem_prompt': mem_prompt,
        'norm_mix': gain((DEPTH, D_MODEL)),
        'w_in': nrm((DEPTH, D_MODEL, IN_COLS), D_MODEL ** -0.5),
        'hg_lb_logits': nrm((DEPTH + 1, HG_WIDTH), 0.5),
        'hg_norm': gain((DEPTH, HG_WIDTH)),
        'w_branch_a': nrm((DEPTH, HG_WIDTH, D_MODEL), HG_WIDTH ** -0.5),
        'w_branch_b': nrm((DEPTH, MB_WIDTH, D_MODEL), MB_WIDTH ** -0.5),
        'w_mix_out': nrm((DEPTH, D_MODEL, D_MODEL), D_MODEL ** -0.5),
        'rel_table': nrm((REL_BUCKETS, MB_HEADS), 0.5),
        'norm_xattn': gain((DEPTH, D_MODEL)),
        'norm_mem': gain((DEPTH, D_MODEL)),
        'w_xq': nrm((DEPTH, D_MODEL, XA_WIDTH), D_MODEL ** -0.5),
        'w_xk': nrm((DEPTH, D_MODEL, XA_WIDTH), D_MODEL ** -0.5),
        'w_xv': nrm((DEPTH, D_MODEL, XA_WIDTH), D_MODEL ** -0.5),
        'w_xo': nrm((DEPTH, XA_WIDTH, D_MODEL), XA_WIDTH ** -0.5),
        'norm_ffn': gain((DEPTH, D_MODEL)),
        'w_group_router': nrm((DEPTH, D_MODEL, N_GROUPS), D_MODEL ** -0.5),
        'b_group_router': nrm((DEPTH, N_GROUPS), 0.01),
        'w_expert_router': nrm((DEPTH, D_MODEL, GE), D_MODEL ** -0.5),
        'b_expert_router': nrm((DEPTH, GE), 0.01),
        'w_expert_gate': nrm((DEPTH, N_GROUPS, EXPERTS_PER_GROUP, D_MODEL, EXPERT_HIDDEN), D_MODEL ** -0.5),
        'w_expert_up': nrm((DEPTH, N_GROUPS, EXPERTS_PER_GROUP, D_MODEL, EXPERT_HIDDEN), D_MODEL ** -0.5),
        'w_expert_down': nrm((DEPTH, N_GROUPS, EXPERTS_PER_GROUP, EXPERT_HIDDEN, D_MODEL), EXPERT_HIDDEN ** -0.5),
        'norm_final': gain((D_MODEL,)),
    }


def reference(x_prompt, x_sample, cache_k, cache_v, state_hgrn, cache_mem_k, cache_mem_v, page_table,
              mem_prompt, norm_mix, w_in, hg_lb_logits, hg_norm, w_branch_a, w_branch_b, w_mix_out,
              rel_table, norm_xattn, norm_mem, w_xq, w_xk, w_xv, w_xo, norm_ffn,
              w_group_router, b_group_router, w_expert_router, b_expert_router,
              w_expert_gate, w_expert_up, w_expert_down, norm_final):
    B = x_prompt.shape[0]
    DB = x_sample.shape[0]
    past_len = page_table.shape[1] * PAGE_SIZE
    lower_bounds = jnp.cumsum(jax.nn.softmax(hg_lb_logits.astype(jnp.float32), axis=0), axis=0)[:DEPTH]
    hp, hs = x_prompt, x_sample
    kp_l, vp_l, sp_l, mkp_l, mvp_l, ks_l, vs_l, ss_l = ([] for _ in range(8))
    for l in range(DEPTH):
        layer_w = (rel_table, lower_bounds[l], norm_mix[l], w_in[l], hg_norm[l], w_branch_a[l],
                   w_branch_b[l], w_mix_out[l], norm_xattn[l], w_xq[l], w_xo[l], norm_ffn[l],
                   w_group_router[l], b_group_router[l], w_expert_router[l], b_expert_router[l],
                   w_expert_gate[l], w_expert_up[l], w_expert_down[l])
        mem_n = rmsnorm(mem_prompt, norm_mem[l])
        mk_p = (mem_n @ w_xk[l]).reshape(B, MEM_LEN, XA_HEADS, XA_DIM)
        mv_p = (mem_n @ w_xv[l]).reshape(B, MEM_LEN, XA_HEADS, XA_DIM)
        s0 = jnp.zeros((B, HG_HEADS, HG_DIM, HG_DIM), jnp.float32)
        hp, kp, vp, sp = decoder_layer(hp, None, None, s0, 0, mk_p, mv_p, *layer_w)
        k_past = cache_k[l][page_table].reshape(DB, past_len, MB_HEADS, MB_DIM)
        v_past = cache_v[l][page_table].reshape(DB, past_len, MB_HEADS, MB_DIM)
        hs, ks, vs, ss = decoder_layer(hs, k_past, v_past, state_hgrn[l], past_len,
                                       cache_mem_k[l], cache_mem_v[l], *layer_w)
        kp_l.append(kp)
        vp_l.append(vp)
        sp_l.append(sp)
        mkp_l.append(mk_p)
        mvp_l.append(mv_p)
        ks_l.append(ks)
        vs_l.append(vs)
        ss_l.append(ss)
    y_prompt = rmsnorm(hp, norm_final)
    y_sample = rmsnorm(hs, norm_final)
    return (y_prompt, y_sample, jnp.stack(kp_l), jnp.stack(vp_l), jnp.stack(sp_l),
            jnp.stack(mkp_l), jnp.stack(mvp_l), jnp.stack(ks_l), jnp.stack(vs_l), jnp.stack(ss_l))
```

```python
import os
import numpy as np
from contextlib import ExitStack
import concourse.bass as bass
import concourse.mybir as mybir
from concourse.bass_utils import run_bass_kernel_spmd

F32 = mybir.dt.float32
BF16 = mybir.dt.bfloat16
I32 = mybir.dt.int32
AF = mybir.ActivationFunctionType
ALU = mybir.AluOpType
AX = mybir.AxisListType

NCORES = 8
D = 1024
T = 2048
NS = 16
TS = 8
NT = T + NS * TS
NTILE = NT // 128
EPS = 1e-6
STAGE = int(os.environ.get('KSTAGE', '9'))
DEBUG = os.environ.get('KDEBUG') == '1'
CW = 1536
LIMIT = int(os.environ.get('KLIMIT', '100000000'))


class Trk:
    __slots__ = ("w", "r", "excl")

    def __init__(self, excl=False):
        self.w = {}
        self.r = {}
        self.excl = excl


class Eng:
    def __init__(self, name, obj, sem):
        self.name, self.o, self.sem = name, obj, sem
        self.cnt = 0
        self.seen = {}

    def wait(self, tok):
        key, sem, val = tok
        if self.seen.get(key, 0) < val:
            self.o.wait_ge(sem, val)
            self.seen[key] = val


class Sched:
    def __init__(self, nc, es):
        self.nc = nc
        self.E = {}
        for name, obj in (("pe", nc.tensor), ("act", nc.scalar), ("dve", nc.vector),
                          ("pool", nc.gpsimd), ("sp", nc.sync)):
            self.E[name] = Eng(name, obj, es.enter_context(nc.semaphore("sem_" + name)))
        self.dsl = {}
        for q, n in (("sp", 24), ("pool", 16), ("act", 8)):
            self.dsl[q] = [[("d%s%d" % (q, i)), es.enter_context(nc.semaphore("d%s%d" % (q, i))), 0]
                           for i in range(n)]
        self.dpos = {"sp": 0, "pool": 0, "act": 0}
        self.nops = 0

    def _deps(self, e, R, W):
        for t in R:
            for tok in t.w.values():
                e.wait(tok)
            if t.excl:
                for tok in t.r.values():
                    if tok[0] != e.name:
                        e.wait(tok)
        for t in W:
            for tok in t.w.values():
                if e.name == "pe" and tok[0] == "pe":
                    continue
                e.wait(tok)
            for tok in t.r.values():
                if e.name == "pe" and tok[0] == "pe":
                    continue
                e.wait(tok)

    def _mark(self, tok, R, W):
        for t in R:
            old = t.r.get(tok[0])
            if old is None or old[2] < tok[2]:
                t.r[tok[0]] = tok
        for t in W:
            t.w = {tok[0]: tok}
            t.r = {}

    def op(self, eng, meth, R, W, *a, **k):
        self.nops += 1
        if self.nops > LIMIT:
            return None
        e = self.E[eng]
        self._deps(e, R, W)
        inst = getattr(e.o, meth)(*a, **k)
        e.cnt += 1
        inst.then_inc(e.sem, 1)
        self._mark((e.name, e.sem, e.cnt), R, W)
        return inst

    def dma(self, q, R, W, meth="dma_start", **k):
        self.nops += 1
        if self.nops > LIMIT:
            return None
        e = self.E[q]
        self._deps(e, R, W)
        slots = self.dsl[q]
        s = slots[self.dpos[q] % len(slots)]
        self.dpos[q] += 1
        e.wait((s[0], s[1], s[2]))
        inst = getattr(e.o, meth)(**k)
        s[2] += 16
        inst.then_inc(s[1], 16)
        self._mark((s[0], s[1], s[2]), R, W)

    def barrier(self):
        toks = [(e.name, e.sem, e.cnt) for e in self.E.values() if e.cnt > 0]
        for q, slots in self.dsl.items():
            for s in slots:
                if s[2] > 0:
                    toks.append((s[0], s[1], s[2]))
        for e in self.E.values():
            for tok in toks:
                e.wait(tok)

    def finish(self):
        for q, slots in self.dsl.items():
            e = self.E[q]
            for s in slots:
                if s[2] > 0:
                    e.wait((s[0], s[1], s[2]))


def host_consts():
    c = np.zeros((128, CW), np.float32)
    c[:, 0:128] = np.eye(128, dtype=np.float32)
    p = np.arange(128)[:, None]
    j = np.arange(128)[None, :]
    c[:, 128:256] = (j >= p).astype(np.float32)
    c[:, 256:384] = ((j >= p) & (j // TS == p // TS)).astype(np.float32)
    c[:, 384:512] = ((j >= p) & (j // 64 == p // 64)).astype(np.float32)
    c[:, 512:1024] = (np.arange(512) % 64 != 0).astype(np.float32)[None, :]
    c[:, 1024:1152] = (np.arange(128) % TS != 0).astype(np.float32)[None, :]
    c[:, 1152:1280] = 1.0 / 128.0
    for jj in range(8):
        c[:, 1280 + jj] = ((np.arange(128) // TS) % 8 == jj).astype(np.float32)
    c[:, 1300] = np.arange(128)
    c[:, 1320:1448] = np.eye(128, dtype=np.float32)[::-1]
    return c


def rel_bucket_np(rel):
    d = np.maximum(rel, 0)
    df = np.maximum(d, 1).astype(np.float32)
    large = 16 + (np.log(df / np.float32(16)) / np.float32(np.log(128 / 16)) * np.float32(16)).astype(np.int32)
    large = np.minimum(large, 31)
    return np.where(d < 16, d, large)


def host_consts2():
    rel = np.arange(768) - 256
    bk = rel_bucket_np(rel)
    c = np.zeros((36, 768), np.float32)
    c[bk, np.arange(768)] = 1.0
    c[0:32, rel < 0] = 0.0
    c[32:36, :] = (rel >= 0).astype(np.float32)[None, :]
    return c


def build_nc(npool=2560):
    nc = bass.Bass("TRN2", target_bir_lowering=False)

    def din(name, shape, dt=F32):
        return nc.dram_tensor(name, list(shape), dt, kind="ExternalInput").ap()

    def dout(name, shape, dt=F32):
        return nc.dram_tensor(name, list(shape), dt, kind="ExternalOutput").ap()

    xall = din("xall", [NT, D])
    memp = din("memp", [256, D])
    consts = din("consts", [128, CW])
    norm_mix = din("norm_mix", [1, D])
    norm_mem = din("norm_mem", [1, D])
    w_in = din("w_in", [D, 5632])
    w_xk = din("w_xk", [D, 512])
    w_xv = din("w_xv", [D, 512])
    lbl = din("lbl", [2, 512])
    hgn_d = din("hg_norm", [1, 512])
    s0_d = din("s0", [NS * 4, 128, 128])
    consts2 = din("consts2", [36, 768])
    rel_d = din("rel_table", [32, 4])
    ckv_d = din("cache_kv", [npool * 128, 1024])
    pt_d = din("page_table", [NS, 16], I32)
    wa_d = din("w_branch_a", [512, D])
    wb_d = din("w_branch_b", [512, D])
    wmix_d = din("w_mix_out", [D, D])
    norm_xa_d = din("norm_xattn", [1, D])
    wxq_d = din("w_xq", [D, 512])
    wxo_d = din("w_xo", [512, D])
    cmk_d = din("cache_mem_k", [NS * 256, 512])
    cmv_d = din("cache_mem_v", [NS * 256, 512])
    norm_ffn_d = din("norm_ffn", [1, D])
    wrg_d = din("w_group_router", [D, 4])
    wre_d = din("w_expert_router", [D, 32])
    brg_d = din("b_group_router", [1, 4])
    bre_d = din("b_expert_router", [1, 32])
    weg_d = din("w_expert_gate", [32, D, 256])
    weu_d = din("w_expert_up", [32, D, 256])
    wed_d = din("w_expert_down", [32, 256, D])
    nfin_d = din("norm_final", [1, D])
    et_scr_t = nc.dram_tensor("et_scr", [4, 768], F32, kind="Internal")
    et_scr = et_scr_t.ap()

    y_o = dout("y", [NT, D])
    k_o = dout("kout", [NT, 512])
    v_o = dout("vout", [NT, 512])
    mk_o = dout("mkout", [256, 512])
    mv_o = dout("mvout", [256, 512])
    sp_o = dout("spout", [4, 128, 128])
    ss_o = dout("ssout", [NS * 4, 128, 128])
    dbg_o = dout("dbg", [128, 4 * NT], BF16) if DEBUG else None

    with ExitStack() as es:
        S = Sched(nc, es)

        cur = [es]

        def sb(name, shape, dt):
            try:
                return cur[0].enter_context(nc.sbuf_tensor(name, list(shape), dt))
            finally:
                if os.environ.get("KALLOC"):
                    nb = int(np.prod(shape[1:])) * (2 if dt == BF16 else 4)
                    print("ALLOC", name, shape, nb)

        cst = sb("cst", [128, CW], F32)
        t_cst = Trk()
        S.dma("sp", [], [t_cst], out=cst[:], in_=consts[:, :])
        ident_bf = sb("ident_bf", [128, 128], BF16)
        t_idb = Trk()
        S.op("dve", "tensor_copy", [t_cst], [t_idb], out=ident_bf[:], in_=cst[:, 0:128])
        g_mix = sb("g_mix", [128, 8], F32)
        g_mem = sb("g_mem", [128, 8], F32)
        t_g = Trk()
        with nc.allow_non_contiguous_dma(reason="tiny gain vectors"):
            S.dma("sp", [], [t_g], out=g_mix[:], in_=norm_mix.rearrange("o (k p) -> p (o k)", p=128))
            S.dma("sp", [], [t_g], out=g_mem[:], in_=norm_mem.rearrange("o (k p) -> p (o k)", p=128))

        PS = [es.enter_context(nc.psum_tensor("ps%d" % i, [128, 512], F32)) for i in range(8)]
        t_ps = [Trk(excl=True) for _ in range(8)]

        memkT = sb("memkT", [128, 4, 256], BF16)
        t_memkT = [Trk() for _ in range(2)]
        memvaug = sb("memvaug", [128, 2, 4, 132], BF16)
        t_memvaug = [Trk() for _ in range(2)]
        stat = sb("stat", [128, 3 * 32], F32)
        t_stat = [Trk() for _ in range(32)]
        RA = sb("RA", [128, 4 * NT + NTILE * 4 * 132], BF16)
        kT = RA[:, 0:4 * NT].rearrange("p (h n) -> p h n", h=4)
        vaug = RA[:, 4 * NT:4 * NT + NTILE * 4 * 132].rearrange("p (t h d) -> p t h d", t=NTILE, h=4)
        mergedT = RA[:, 0:8 * NT].rearrange("p (k n) -> p k n", k=8)
        t_mrg = [Trk() for _ in range(NTILE)]
        t_kT = [Trk() for _ in range(NTILE)]
        t_vaug = [Trk() for _ in range(NTILE)]
        esM = ExitStack()
        cur[0] = esM
        xnT = sb("xnT", [128, 8, NT], BF16)
        t_xnT = [Trk() for _ in range(NTILE)]
        oaT = sb("oaT", [128, 4, NT], BF16)
        t_oaT = [[Trk() for _ in range(5)] for _ in range(4)]
        obT = sb("obT", [128, 4, NT], BF16)
        t_obT = [Trk() for _ in range(NTILE)]
        esA = ExitStack()
        cur[0] = esA
        vhg = sb("vhg", [128, NTILE, 512], BF16)
        t_vhg = [Trk() for _ in range(NTILE)]
        esB = ExitStack()
        cur[0] = esB
        memnT = sb("memnT", [128, 8, 256], BF16)
        t_memnT = [Trk() for _ in range(2)]
        xt = [sb("xt%d" % i, [128, D], F32) for i in range(2)]
        t_xt = [Trk() for _ in range(2)]
        junk = sb("junk", [128, D], BF16)
        t_junk = Trk()
        xnb = [sb("xnb%d" % i, [128, D], BF16) for i in range(2)]
        t_xnb = [Trk() for _ in range(2)]

        cnt = {"ps": 0, "x": 0, "st": 0, "lps": 0}

        def next_ps():
            i = 2 + cnt["ps"] % 6
            cnt["ps"] += 1
            return PS[i], t_ps[i]

        def long_ps():
            i = cnt["lps"] % 2
            cnt["lps"] += 1
            return PS[i], t_ps[i]

        def norm_T(src_rows, gain, dstT, dcol, t_dst):
            i = cnt["x"] % 2
            cnt["x"] += 1
            si = cnt["st"] % 32
            cnt["st"] += 1
            S.dma("sp", [], [t_xt[i]], out=xt[i][:], in_=src_rows)
            ts_ = t_stat[si]
            S.op("act", "activation", [t_xt[i]], [t_junk, ts_], out=junk[:], in_=xt[i][:], func=AF.Square,
                 accum_out=stat[:, si:si + 1])
            S.op("act", "activation", [ts_], [ts_], out=stat[:, 32 + si:33 + si], in_=stat[:, si:si + 1],
                 func=AF.Ln, scale=1.0 / D, bias=EPS)
            S.op("act", "activation", [ts_], [ts_], out=stat[:, 64 + si:65 + si], in_=stat[:, 32 + si:33 + si],
                 func=AF.Exp, scale=-0.5)
            S.op("dve", "tensor_scalar", [t_xt[i], ts_], [t_xnb[i]], out=xnb[i][:], in0=xt[i][:],
                 scalar1=stat[:, 64 + si:65 + si], scalar2=None, op0=ALU.mult)
            ps, tp = next_ps()
            psb = ps[:].bitcast(BF16)
            for k in range(8):
                S.op("pe", "transpose", [t_xnb[i], t_idb], [tp], out=psb[:, k * 128:(k + 1) * 128],
                     in_=xnb[i][:, k * 128:(k + 1) * 128], identity=ident_bf[:])
            S.op("dve", "tensor_tensor", [tp, t_g], [t_dst], out=dstT[:, :, dcol:dcol + 128],
                 in0=psb.rearrange("p (k n) -> p k n", k=8),
                 in1=gain[:, :].unsqueeze(2).to_broadcast([128, 8, 128]), op=ALU.mult)

        wmem = sb("wmem", [128, 8, 1024], BF16)
        t_wmem = [Trk() for _ in range(2)]
        for c, wsrc in enumerate((w_xk, w_xv)):
            for k in range(8):
                S.dma("pool", [], [t_wmem[c]], out=wmem[:, k, c * 512:(c + 1) * 512],
                      in_=wsrc[k * 128:(k + 1) * 128, :])

        wtm = sb("wtm", [128, 8, 1536], BF16)
        t_wtm = [Trk() for _ in range(3)]
        for c, c0 in enumerate((1024, 2560, 3072)):
            for k in range(8):
                S.dma("pool", [], [t_wtm[c]], out=wtm[:, k, c * 512:(c + 1) * 512],
                      in_=w_in[k * 128:(k + 1) * 128, c0:c0 + 512])
        t_ones = Trk()
        for tt in range(NTILE):
            S.op("pool", "memset", [], [t_vaug[tt]], vaug[:, tt, :, 128:132], 1.0)
        for tt in range(2):
            S.op("pool", "memset", [], [t_memvaug[tt]], memvaug[:, tt, :, 128:132], 1.0)

        kst = [sb("kst%d" % i, [128, 512], F32) for i in range(2)]
        t_kst = [Trk() for _ in range(2)]
        vst = [sb("vst%d" % i, [128, 512], F32) for i in range(2)]
        t_vst = [Trk() for _ in range(2)]
        kbf = [sb("kbf%d" % i, [128, 512], BF16) for i in range(2)]
        t_kbf = [Trk() for _ in range(2)]
        cnt["kv"] = 0

        def proj_kv(srcT, col0, t_src, w, wc_k, wc_v, t_wk, t_wv, k_dst, v_dst, kT_dst, kcol, t_kTd,
                    vaug_dst, t_vaugd):
            i = cnt["kv"] % 2
            cnt["kv"] += 1
            psk, tpk = next_ps()
            for k in range(8):
                S.op("pe", "matmul", [t_src, t_wk], [tpk], psk[:, :], lhsT=srcT[:, k, col0:col0 + 128],
                     rhs=w[:, k, wc_k:wc_k + 512], start=(k == 0), stop=(k == 7))
            psv, tpv = next_ps()
            for k in range(8):
                S.op("pe", "matmul", [t_src, t_wv], [tpv], psv[:, :], lhsT=srcT[:, k, col0:col0 + 128],
                     rhs=w[:, k, wc_v:wc_v + 512], start=(k == 0), stop=(k == 7))
            S.op("act", "activation", [tpk], [t_kst[i]], out=kst[i][:], in_=psk[:, :], func=AF.Copy)
            S.op("dve", "tensor_copy", [t_kst[i]], [t_kbf[i]], out=kbf[i][:], in_=kst[i][:])
            S.dma("pool", [t_kst[i]], [], out=k_dst, in_=kst[i][:])
            S.op("act", "activation", [tpv], [t_vst[i]], out=vst[i][:], in_=psv[:, :], func=AF.Copy)
            S.op("dve", "tensor_copy", [t_vst[i]], [t_vaugd], out=vaug_dst,
                 in_=vst[i][:].rearrange("p (h d) -> p h d", h=4))
            S.dma("pool", [t_vst[i]], [], out=v_dst, in_=vst[i][:])
            pst, tpt = next_ps()
            pstb = pst[:].bitcast(BF16)
            for h in range(4):
                S.op("pe", "transpose", [t_kbf[i], t_idb], [tpt], out=pstb[:, h * 128:(h + 1) * 128],
                     in_=kbf[i][:, h * 128:(h + 1) * 128], identity=ident_bf[:])
            S.op("act", "activation", [tpt], [t_kTd], out=kT_dst[:, :, kcol:kcol + 128],
                 in_=pstb[:, 0:512].rearrange("p (h n) -> p h n", h=4), func=AF.Copy)

        for m in range(2):
            norm_T(memp[m * 128:(m + 1) * 128, :], g_mem, memnT, m * 128, t_memnT[m])
            proj_kv(memnT, m * 128, t_memnT[m], wmem, 0, 512, t_wmem[0], t_wmem[1],
                    mk_o[m * 128:(m + 1) * 128, :], mv_o[m * 128:(m + 1) * 128, :],
                    memkT, m * 128, t_memkT[m], memvaug[:, m, :, 0:128], t_memvaug[m])

        norm_T(xall[0:128, :], g_mix, xnT, 0, t_xnT[0])
        for tt in range(NTILE):
            if tt + 1 < NTILE:
                norm_T(xall[(tt + 1) * 128:(tt + 2) * 128, :], g_mix, xnT, (tt + 1) * 128, t_xnT[tt + 1])
            proj_kv(xnT, tt * 128, t_xnT[tt], wtm, 512, 1024, t_wtm[1], t_wtm[2],
                    k_o[tt * 128:(tt + 1) * 128, :], v_o[tt * 128:(tt + 1) * 128, :],
                    kT, tt * 128, t_kT[tt], vaug[:, tt, :, 0:128], t_vaug[tt])
            psh, tph = next_ps()
            for k in range(8):
                S.op("pe", "matmul", [t_xnT[tt], t_wtm[0]], [tph], psh[:, :],
                     lhsT=xnT[:, k, tt * 128:(tt + 1) * 128], rhs=wtm[:, k, 0:512],
                     start=(k == 0), stop=(k == 7))
            S.op("act", "activation", [tph], [t_vhg[tt]], out=vhg[:, tt, :], in_=psh[:, :], func=AF.Copy)

        S.barrier()
        esB.close()
        esC = ExitStack()
        cur[0] = esC
        if STAGE >= 2:
            lraw = sb("lraw", [128, 8], F32)
            lb = sb("lb", [128, 16], F32)
            hgn = sb("hgn", [128, 4], F32)
            t_lb = Trk()
            with nc.allow_non_contiguous_dma(reason="tiny vectors"):
                S.dma("sp", [], [t_lb], out=lraw[:, :].rearrange("p (s h) -> p s h", s=2),
                      in_=lbl.rearrange("s (h p) -> p s h", p=128))
                S.dma("sp", [], [t_lb], out=hgn[:], in_=hgn_d.rearrange("o (h p) -> p (o h)", p=128))
            S.op("dve", "tensor_tensor", [t_lb], [t_lb], out=lb[:, 0:4], in0=lraw[:, 0:4], in1=lraw[:, 4:8],
                 op=ALU.subtract)
            S.op("act", "activation", [t_lb], [t_lb], out=lb[:, 4:8], in_=lb[:, 0:4], func=AF.Sigmoid)
            S.op("dve", "tensor_scalar", [t_lb], [t_lb], out=lb[:, 8:12], in0=lb[:, 4:8], scalar1=-1.0,
                 scalar2=1.0, op0=ALU.mult, op1=ALU.add)
            S.op("dve", "tensor_scalar", [t_lb], [t_lb], out=lb[:, 12:16], in0=lb[:, 8:12], scalar1=-1.0,
                 scalar2=None, op0=ALU.mult)
            mask64 = sb("mask64", [128, 128], F32)
            t_m = Trk()
            whg = sb("whg", [128, 8, 1536], BF16)
            t_whg = [Trk() for _ in range(3)]
            for c, c0 in enumerate((0, 512, 1536)):
                for k in range(8):
                    S.dma("pool", [], [t_whg[c]], out=whg[:, k, c * 512:(c + 1) * 512],
                          in_=w_in[k * 128:(k + 1) * 128, c0:c0 + 512])
            def wt(name, dt=F32, n=512):
                return sb(name, [128, n], dt), Trk()
            sig, t_sig = wt("h_sig")
            ff, t_ff = wt("h_f")
            bb, t_bb = wt("h_b")
            qs, t_qs = wt("h_qs")
            H4 = range(4)
            eb, t_eb = zip(*[wt("h_eb%d" % h) for h in H4])
            gs, t_gs = zip(*[wt("h_gs%d" % h, BF16) for h in H4])
            Qt, t_Qt = zip(*[wt("h_Qt%d" % h, BF16) for h in H4])
            Kt, t_Kt = zip(*[wt("h_Kt%d" % h, BF16) for h in H4])
            KhT, t_KhT = zip(*[wt("h_KhT%d" % h, BF16) for h in H4])
            Qt32, t_Qt32 = zip(*[wt("h_Qt32_%d" % h, F32, 128) for h in H4])
            Kh = [[sb("h_Kh%d_%d" % (h, i), [128, 128], BF16) for i in range(2)] for h in H4]
            t_Kh = [[Trk() for _ in range(2)] for _ in H4]
            sTm = [[sb("h_sTm%d_%d" % (h, i), [128, 128], BF16) for i in range(2)] for h in H4]
            t_sTm = [[Trk() for _ in range(2)] for _ in H4]
            Sf = [sb("h_S%d" % h, [128, 128], F32) for h in H4]
            Sb = [sb("h_Sb%d" % h, [128, 128], BF16) for h in H4]
            t_S = [Trk() for _ in H4]
            t_Sb = [Trk() for _ in H4]
            s0t = [sb("h_s0_%d" % i, [128, 128], F32) for i in range(8)]
            t_s0 = [Trk() for _ in range(8)]
            snew = [sb("h_sn_%d" % i, [128, 128], F32) for i in range(8)]
            t_sn = [Trk() for _ in range(8)]
            vm = sb("h_vm", [128, 8, 128], BF16)
            t_vm = Trk()
            cnt["s0"] = 0
            cnt["hg"] = 0

            def hg_ps():
                i = 4 + cnt["hg"] % 4
                cnt["hg"] += 1
                return PS[i], t_ps[i]

            for h in H4:
                S.op("dve", "memset", [], [t_S[h]], Sf[h][:], 0.0)
                S.op("pool", "memset", [], [t_Sb[h]], Sb[h][:], 0.0)
            for g in range(5):
                c0 = g * 512
                n = 512 if g < 4 else 128
                tsrc = [t_xnT[tt] for tt in range(g * 4, min(g * 4 + 4, NTILE))]
                C = 64 if g < 4 else TS
                for h in H4:
                    pss = []
                    for c in range(3):
                        ps, tp = hg_ps()
                        for k in range(8):
                            S.op("pe", "matmul", tsrc + [t_whg[c]], [tp], ps[:, 0:n],
                                 lhsT=whg[:, k, c * 512 + h * 128:c * 512 + (h + 1) * 128],
                                 rhs=xnT[:, k, c0:c0 + n], start=(k == 0), stop=(k == 7))
                        pss.append((ps, tp))
                    (psq, tpq), (psf, tpf), (psg, tpg) = pss
                    S.op("act", "activation", [tpf], [t_sig], out=sig[:, 0:n], in_=psf[:, 0:n], func=AF.Sigmoid)
                    S.op("act", "activation", [tpq], [t_qs], out=qs[:, 0:n], in_=psq[:, 0:n], func=AF.Silu)
                    S.op("act", "activation", [tpg], [t_gs[h]], out=gs[h][:, 0:n], in_=psg[:, 0:n], func=AF.Silu)
                    S.op("dve", "tensor_scalar", [t_sig, t_lb], [t_ff], out=ff[:, 0:n], in0=sig[:, 0:n],
                         scalar1=lb[:, 8 + h:9 + h], scalar2=lb[:, 4 + h:5 + h], op0=ALU.mult, op1=ALU.add)
                    S.op("dve", "tensor_scalar", [t_sig, t_lb], [t_sig], out=sig[:, 0:n], in0=sig[:, 0:n],
                         scalar1=lb[:, 12 + h:13 + h], scalar2=lb[:, 8 + h:9 + h], op0=ALU.mult, op1=ALU.add)
                    S.op("act", "activation", [t_ff], [t_ff], out=ff[:, 0:n], in_=ff[:, 0:n], func=AF.Ln)
                    rm = cst[:, 512:1024] if g < 4 else cst[:, 1024:1152]
                    S.op("dve", "tensor_tensor_scan", [t_ff, t_cst], [t_bb], out=bb[:, 0:n], data0=rm,
                         data1=ff[:, 0:n], initial=0.0, op0=ALU.mult, op1=ALU.add)
                    S.op("act", "activation", [t_bb], [t_eb[h]], out=eb[h][:, 0:n], in_=bb[:, 0:n], func=AF.Exp)
                    S.op("act", "activation", [t_bb], [t_ff], out=ff[:, 0:n], in_=bb[:, 0:n], func=AF.Exp,
                         scale=-1.0)
                    S.op("dve", "tensor_tensor", [t_qs, t_eb[h]], [t_Qt[h]], out=Qt[h][:, 0:n], in0=qs[:, 0:n],
                         in1=eb[h][:, 0:n], op=ALU.mult)
                    S.op("dve", "tensor_tensor", [t_sig, t_ff], [t_Kt[h]], out=Kt[h][:, 0:n], in0=sig[:, 0:n],
                         in1=ff[:, 0:n], op=ALU.mult)
                    for ci in range(n // C):
                        S.op("dve", "tensor_scalar", [t_Kt[h], t_eb[h]], [t_KhT[h]], out=KhT[h][:, ci * C:(ci + 1) * C],
                             in0=Kt[h][:, ci * C:(ci + 1) * C], scalar1=eb[h][:, (ci + 1) * C - 1:(ci + 1) * C],
                             scalar2=None, op0=ALU.mult)
                    if g == 4:
                        S.op("dve", "tensor_tensor", [t_qs, t_eb[h]], [t_Qt32[h]], out=Qt32[h][:, 0:128], in0=qs[:, 0:128],
                             in1=eb[h][:, 0:128], op=ALU.mult)
                pso = [PS[h] for h in H4]
                tpo = [t_ps[h] for h in H4]
                for ti in range(n // 128):
                    tt = g * 4 + ti
                    cs = ti * 128
                    ki = ti % 2
                    for h in H4:
                        pst, tpt = hg_ps()
                        pstb = pst[:].bitcast(BF16)
                        S.op("pe", "transpose", [t_KhT[h], t_idb], [tpt], out=pstb[:, 0:128],
                             in_=KhT[h][:, cs:cs + 128], identity=ident_bf[:])
                        S.op("act", "activation", [tpt], [t_Kh[h][ki]], out=Kh[h][ki][:], in_=pstb[:, 0:128], func=AF.Copy)
                        pss_, tps = hg_ps()
                        S.op("pe", "matmul", [t_Kt[h], t_Qt[h]], [tps], pss_[:, 0:128], lhsT=Kt[h][:, cs:cs + 128],
                             rhs=Qt[h][:, cs:cs + 128], start=True, stop=True)
                        mk_ = cst[:, 384:512] if g < 4 else cst[:, 256:384]
                        S.op("dve", "tensor_tensor", [tps, t_cst], [t_sTm[h][ki]], out=sTm[h][ki][:], in0=pss_[:, 0:128],
                             in1=mk_, op=ALU.mult)
                        S.op("pe", "matmul", [t_vhg[tt], t_sTm[h][ki]], [tpo[h]], pso[h][:, cs:cs + 128],
                             lhsT=vhg[:, tt, h * 128:(h + 1) * 128], rhs=sTm[h][ki][:], start=True, stop=False)
                    if g < 4:
                        for half in range(2):
                            r0 = half * 64
                            for h in H4:
                                S.op("pe", "matmul", [t_Sb[h], t_Qt[h]], [tpo[h]], pso[h][:, cs + r0:cs + r0 + 64], lhsT=Sb[h][:],
                                     rhs=Qt[h][:, cs + r0:cs + r0 + 64], start=False, stop=(half == 1))
                                psu, tpu = hg_ps()
                                S.op("pe", "matmul", [t_Kh[h][ki], t_vhg[tt]], [tpu], psu[:, 0:128],
                                     lhsT=Kh[h][ki][r0:r0 + 64, :], rhs=vhg[r0:r0 + 64, tt, h * 128:(h + 1) * 128],
                                     start=True, stop=True)
                                S.op("dve", "scalar_tensor_tensor", [t_S[h], t_eb[h], tpu], [t_S[h]], out=Sf[h][:], in0=Sf[h][:],
                                     scalar=eb[h][:, cs + r0 + 63:cs + r0 + 64], in1=psu[:, 0:128], op0=ALU.mult,
                                     op1=ALU.add)
                                S.op("act", "activation", [t_S[h]], [t_Sb[h]], out=Sb[h][:], in_=Sf[h][:], func=AF.Copy)
                    else:
                        for h in H4:
                            for jj in range(8):
                                S.op("dve", "tensor_scalar", [t_vhg[tt], t_cst], [t_vm], out=vm[:, jj, :],
                                     in0=vhg[:, tt, h * 128:(h + 1) * 128], scalar1=cst[:, 1280 + jj:1281 + jj],
                                     scalar2=None, op0=ALU.mult)
                            for i in range(NS):
                                bi = cnt["s0"] % 8
                                cnt["s0"] += 1
                                S.dma("sp", [], [t_s0[bi]], out=s0t[bi][:], in_=s0_d[i * 4 + h])
                                S.op("pe", "matmul", [t_s0[bi], t_Qt32[h]], [tpo[h]], pso[h][:, i * TS:(i + 1) * TS],
                                     lhsT=s0t[bi][:], rhs=Qt32[h][:, i * TS:(i + 1) * TS], start=False,
                                     stop=(i == NS - 1))
                                psu, tpu = hg_ps()
                                g32 = (i // 8) * 64
                                S.op("pe", "matmul", [t_Kh[h][ki], t_vm], [tpu], psu[:, 0:128],
                                     lhsT=Kh[h][ki][g32:g32 + 64, :], rhs=vm[g32:g32 + 64, i % 8, :],
                                     start=True, stop=True)
                                S.op("dve", "scalar_tensor_tensor", [t_s0[bi], t_eb[h], tpu], [t_sn[bi]],
                                     out=snew[bi][:], in0=s0t[bi][:], scalar=eb[h][:, i * TS + TS - 1:i * TS + TS],
                                     in1=psu[:, 0:128], op0=ALU.mult, op1=ALU.add)
                                S.dma("pool", [t_sn[bi]], [], out=ss_o[i * 4 + h], in_=snew[bi][:])
                if g == 3:
                    for h in H4:
                        S.dma("pool", [t_S[h]], [], out=sp_o[h], in_=Sf[h][:])
                for h in H4:
                    S.op("act", "activation", [tpo[h]], [t_sig], out=sig[:, 0:n], in_=pso[h][:, 0:n], func=AF.Square)
                    psm, tpm = hg_ps()
                    S.op("pe", "matmul", [t_sig, t_cst], [tpm], psm[:, 0:n], lhsT=cst[:, 1152:1280], rhs=sig[:, 0:n],
                         start=True, stop=True)
                    S.op("act", "activation", [tpm], [t_bb], out=bb[:, 0:n], in_=psm[:, 0:n], func=AF.Ln,
                         bias=EPS)
                    S.op("act", "activation", [t_bb], [t_bb], out=bb[:, 0:n], in_=bb[:, 0:n], func=AF.Exp,
                         scale=-0.5)
                    S.op("dve", "tensor_tensor", [tpo[h], t_bb], [t_ff], out=ff[:, 0:n], in0=pso[h][:, 0:n],
                         in1=bb[:, 0:n], op=ALU.mult)
                    S.op("dve", "scalar_tensor_tensor", [t_ff, t_lb, t_gs[h]], [t_oaT[h][g]], out=oaT[:, h, c0:c0 + n],
                         in0=ff[:, 0:n], scalar=hgn[:, h:h + 1], in1=gs[h][:, 0:n], op0=ALU.mult, op1=ALU.mult)

        S.barrier()
        esC.close()
        esA.close()
        cur[0] = esM

        esD = ExitStack()
        cur[0] = esD
        if STAGE >= 3:
            SCALE = float(128 ** -0.5)
            Eown = sb("Eown", [128, 4, 512], F32)
            Eadj = sb("Eadj", [128, 4, 512], F32)
            cfar = sb("cfar", [128, 4], F32)
            EownS = sb("EownS", [128, 4, 128], F32)
            esD0 = ExitStack()
            cur[0] = esD0
            relt = sb("relt", [32, 4], F32)
            oh = sb("oh", [32, 768], F32)
            crow = sb("crow", [4, 768], F32)
            et = sb("et", [4, 768], F32)
            t_rel, t_oh, t_et, t_scr, t_E = Trk(), Trk(), Trk(), Trk(), Trk()
            with nc.allow_non_contiguous_dma(reason="tiny table"):
                S.dma("sp", [], [t_rel], out=relt[:], in_=rel_d[:, :])
            S.dma("sp", [], [t_oh], out=oh[:], in_=consts2[0:32, :])
            S.dma("sp", [], [t_oh], out=crow[:], in_=consts2[32:36, :])
            for half in range(2):
                ps, tp = next_ps()
                S.op("pe", "matmul", [t_rel, t_oh], [tp], ps[0:4, 0:384], lhsT=relt[:, :],
                     rhs=oh[:, half * 384:(half + 1) * 384], start=True, stop=True)
                S.op("act", "activation", [tp], [t_et], out=et[:, half * 384:(half + 1) * 384], in_=ps[0:4, 0:384],
                     func=AF.Exp)
            S.op("dve", "tensor_tensor", [t_et, t_oh], [t_et], out=et[:], in0=et[:], in1=crow[:], op=ALU.mult)
            S.dma("sp", [t_et], [t_scr], out=et_scr[:, :], in_=et[:])
            hk = [sb("hk%d" % i, [128, 256], F32) for i in range(2)]
            t_hk = [Trk() for _ in range(2)]
            cnt["hk"] = 0
            with nc.allow_non_contiguous_dma(reason="toeplitz expansion of the bias vector"):
                for h in range(4):
                    for (Edst, cbase) in ((Eown, 256), (Eadj, 512)):
                        for j in range(2):
                            bi = cnt["hk"] % 2
                            cnt["hk"] += 1
                            S.dma("sp", [t_scr], [t_hk[bi]], out=hk[bi][:],
                                  in_=bass.AP(et_scr_t, h * 768 + cbase - 128 * j - 127, [[1, 128], [1, 256]]))
                            ps, tp = next_ps()
                            S.op("pe", "matmul", [t_hk[bi], t_cst], [tp], ps[:, 0:256], lhsT=cst[:, 1320:1448],
                                 rhs=hk[bi][:], start=True, stop=True)
                            S.op("act", "activation", [tp], [t_E], out=Edst[:, h, j * 256:(j + 1) * 256],
                                 in_=ps[:, 0:256], func=AF.Copy)
                    S.dma("sp", [t_scr], [t_E], out=cfar[:, h:h + 1],
                          in_=bass.AP(et_scr_t, h * 768 + 767, [[0, 128], [1, 1]]))
            for h in range(4):
                S.op("dve", "tensor_tensor", [t_E, t_cst], [t_E], out=EownS[:, h, :], in0=Eown[:, h, 0:128],
                     in1=cst[:, 256:384], op=ALU.mult)
            S.barrier()
            esD0.close()
            cur[0] = esD
            ptb = sb("ptb", [128, NS * 16], I32)
            idx = sb("idx", [128, NS * 16], I32)
            t_idx = Trk()
            with nc.allow_non_contiguous_dma(reason="page table broadcast"):
                S.dma("sp", [], [t_idx], out=ptb[:], in_=bass.AP(pt_d.tensor, 0, [[0, 128], [1, NS * 16]]))
            S.op("dve", "tensor_scalar", [t_idx, t_cst], [t_idx], out=idx[:], in0=ptb[:], scalar1=128.0,
                 scalar2=cst[:, 1300:1301], op0=ALU.mult, op1=ALU.add)
            wq = sb("wq", [128, 8, 512], BF16)
            t_wq = Trk()
            for k in range(8):
                S.dma("pool", [], [t_wq], out=wq[:, k, :], in_=w_in[k * 128:(k + 1) * 128, 2048:2560])
            qT = sb("qT", [128, NT], BF16)
            t_qT = [Trk() for _ in range(5)]
            qTs = sb("qTs", [128, 4, 128], BF16)
            pTo = sb("pTo", [128, 4, 128], BF16)
            t_qTs, t_pTo = Trk(), Trk()
            km32 = sb("km32", [128, 8], F32)
            kmb = sb("kmb", [128, 8], BF16)
            t_km = Trk()
            gsb = sb("gsb", [128, 8], F32)
            m8 = sb("m8", [128, 8], F32)
            selw = [sb("selw%d" % i, [128, 16], F32) for i in range(2)]
            t_sel = [Trk() for _ in range(2)]
            t_gs2 = Trk()
            acc = [sb("acc%d" % i, [128, 132], F32) for i in range(2)]
            t_acc = [Trk() for _ in range(2)]
            pT = [sb("pT%d" % i, [128, 512], BF16) for i in range(2)]
            t_pT = [Trk() for _ in range(2)]
            pTf = sb("pTf", [128, 512], F32)
            t_pTf = Trk()
            obt = sb("obt", [128, 128], BF16)
            rden = sb("rden", [128, 1], F32)
            t_obt = Trk()
            cnt["pT"] = 0
            alltk = list(t_kT[0:16])
            pTf2 = sb("pTf2", [128, 64], F32)
            t_pTf2 = Trk()

            def mb_ps():
                i = 3 + cnt["ps"] % 5
                cnt["ps"] += 1
                return PS[i], t_ps[i]
            next_ps = mb_ps
            for h in range(4):
                ps, tp = next_ps()
                for k in range(8):
                    S.op("pe", "matmul", [t_xnT[16], t_wq], [tp], ps[:, 0:128], lhsT=wq[:, k, h * 128:(h + 1) * 128],
                         rhs=xnT[:, k, 2048:2176], start=(k == 0), stop=(k == 7))
                S.op("act", "activation", [tp], [t_qTs], out=qTs[:, h, :], in_=ps[:, 0:128], func=AF.Copy)
                ps, tp = next_ps()
                S.op("pe", "matmul", [t_kT[16], t_qTs], [tp], ps[:, 0:128], lhsT=kT[:, h, 2048:2176],
                     rhs=qTs[:, h, :], start=True, stop=True)
                S.op("act", "activation", [tp], [t_pTf], out=pTf[:, 0:128], in_=ps[:, 0:128], func=AF.Exp, scale=SCALE)
                S.op("dve", "tensor_tensor", [t_pTf, t_E], [t_pTo], out=pTo[:, h, :], in0=pTf[:, 0:128],
                     in1=EownS[:, h, :], op=ALU.mult)
            kvpg = [sb("kvpg%d" % i, [128, 1024], F32) for i in range(3)]
            t_kvpg = [Trk() for _ in range(3)]
            kTs = sb("kTs", [128, 4, 2048], BF16)
            t_kTs = [Trk() for _ in range(16)]
            vaugs = sb("vaugs", [128, 16, 4, 132], BF16)
            t_vaugs = [Trk() for _ in range(16)]
            for pg in range(16):
                S.op("pool", "memset", [], [t_vaugs[pg]], vaugs[:, pg, :, 128:132], 1.0)
            kms32 = sb("kms32", [128, 32], F32)
            kmsb = sb("kmsb", [128, 32], BF16)
            t_kms = Trk()
            gss = sb("gss", [8, 32], F32)
            m8s = sb("m8s", [8, 8], F32)
            ws = sb("ws", [8, 64], F32)
            t_ws = Trk()
            pTs = sb("pTs", [128, 512], BF16)
            t_pTs = Trk()
            accs = sb("accs", [8, 4, 132], F32)
            t_accs = Trk()
            obs = sb("obs", [8, 4, 128], BF16)
            rdens = sb("rdens", [8, 4], F32)
            t_obs = Trk()
            def gen_sample():
                def gather(f):
                    S.dma("pool", [t_idx], [t_kvpg[f % 3]], meth="indirect_dma_start", out=kvpg[f % 3][:], out_offset=None,
                          in_=ckv_d, in_offset=bass.IndirectOffsetOnAxis(idx[:, f:f + 1], 0))
                gather(0)
                gather(1)
                for i in range(NS):
                    for pg in range(16):
                        f = i * 16 + pg
                        bi = f % 3
                        if f + 2 < NS * 16:
                            gather(f + 2)
                        ps, tp = next_ps()
                        for h in range(4):
                            S.op("pe", "transpose", [t_kvpg[bi], t_cst], [tp], out=ps[:, h * 128:(h + 1) * 128],
                                 in_=kvpg[bi][:, h * 128:(h + 1) * 128], identity=cst[:, 0:128])
                        S.op("act", "activation", [tp], [t_kTs[pg]], out=kTs[:, :, pg * 128:(pg + 1) * 128],
                             in_=ps[:, :].rearrange("p (h n) -> p h n", h=4), func=AF.Copy)
                        S.op("dve", "tensor_copy", [t_kvpg[bi]], [t_vaugs[pg]], out=vaugs[:, pg, :, 0:128],
                             in_=kvpg[bi][:, 512:1024].rearrange("p (h d) -> p h d", h=4))
                        yield
                    S.op("dve", "tensor_reduce", t_kTs, [t_kms], out=kms32[:, :],
                         in_=kTs[:, :, :].rearrange("p h (n k) -> p (h n) k", n=8), axis=AX.X, op=ALU.add)
                    S.op("dve", "tensor_copy", [t_kms], [t_kms], out=kmsb[:], in_=kms32[:])
                    ps, tp = next_ps()
                    for h in range(4):
                        S.op("pe", "matmul", [t_qTs, t_kms], [tp], ps[0:8, h * 8:(h + 1) * 8],
                             lhsT=qTs[:, h, i * TS:(i + 1) * TS], rhs=kmsb[:, h * 8:(h + 1) * 8], start=True, stop=True)
                    S.op("dve", "tensor_copy", [tp], [t_ws], out=gss[:], in_=ps[0:8, 0:32])
                    for h in range(4):
                        S.op("dve", "max", [t_ws], [t_ws], out=m8s[:], in_=gss[:, h * 8:(h + 1) * 8])
                        S.op("dve", "tensor_scalar", [t_ws], [t_ws], out=ws[:, h * 8:(h + 1) * 8], in0=gss[:, h * 8:(h + 1) * 8],
                             scalar1=m8s[:, 2:3], scalar2=None, op0=ALU.is_ge)
                        S.op("dve", "tensor_scalar", [t_ws, t_E], [t_ws], out=ws[:, 32 + h * 8:40 + h * 8],
                             in0=ws[:, h * 8:(h + 1) * 8], scalar1=cfar[0:8, h:h + 1], scalar2=None, op0=ALU.mult)
                    yield
                    pss, tps = PS[2], t_ps[2]
                    for pg in range(16):
                        for h in range(4):
                            cc = (pg * 4 + h) * TS
                            S.op("pe", "matmul", [t_kTs[pg], t_qTs], [tps], pss[:, cc:cc + TS],
                                 lhsT=kTs[:, h, pg * 128:(pg + 1) * 128], rhs=qTs[:, h, i * TS:(i + 1) * TS],
                                 start=True, stop=True)
                        if pg % 4 == 3:
                            yield
                    S.op("act", "activation", [tps], [t_pTs], out=pTs[:, 0:448], in_=pss[:, 0:448], func=AF.Exp, scale=SCALE)
                    S.op("act", "activation", [tps], [t_pTf2], out=pTf2[:, 0:64], in_=pss[:, 448:512], func=AF.Exp,
                         scale=SCALE)
                    S.op("dve", "tensor_tensor", [t_pTf2, t_E], [t_pTs],
                         out=pTs[:, 448:512].rearrange("p (j h q) -> p j h q", j=2, h=4),
                         in0=pTf2[:, 0:64].rearrange("p (j h q) -> p j h q", j=2, h=4),
                         in1=Eadj[:, :, :].rearrange("p h (j q) -> p j h q", j=2)[:, :, :, 0:TS], op=ALU.mult)
                    yield
                    for h in range(4):
                        for nb in range(8):
                            po, tpo2 = next_ps()
                            for j in range(2):
                                pg = 2 * nb + j
                                cc = (pg * 4 + h) * TS
                                S.op("pe", "matmul", [t_pTs, t_vaugs[pg]], [tpo2], po[0:8, 0:129], lhsT=pTs[:, cc:cc + TS],
                                     rhs=vaugs[:, pg, h, 0:129], start=(j == 0), stop=(j == 1))
                            w = ws[:, h * 8 + nb:h * 8 + nb + 1] if nb == 7 else ws[:, 32 + h * 8 + nb:33 + h * 8 + nb]
                            if nb == 0:
                                S.op("dve", "tensor_scalar", [tpo2, t_ws], [t_accs], out=accs[:, h, 0:129], in0=po[0:8, 0:129],
                                     scalar1=w, scalar2=None, op0=ALU.mult)
                            else:
                                S.op("dve", "scalar_tensor_tensor", [tpo2, t_ws, t_accs], [t_accs], out=accs[:, h, 0:129],
                                     in0=po[0:8, 0:129], scalar=w, in1=accs[:, h, 0:129], op0=ALU.mult, op1=ALU.add)
                        po, tpo2 = next_ps()
                        S.op("pe", "matmul", [t_pTo, t_vaug[16]], [tpo2], po[0:8, 0:129], lhsT=pTo[:, h, i * TS:(i + 1) * TS],
                             rhs=vaug[:, 16, h, 0:129], start=True, stop=True)
                        S.op("dve", "tensor_tensor", [tpo2, t_accs], [t_accs], out=accs[:, h, 0:129], in0=po[0:8, 0:129],
                             in1=accs[:, h, 0:129], op=ALU.add)
                        yield
                    S.op("dve", "reciprocal", [t_accs], [t_obs], out=rdens[:, :], in_=accs[:, :, 128])
                    S.op("dve", "tensor_tensor", [t_accs, t_obs], [t_obs], out=obs[:, :, :], in0=accs[:, :, 0:128],
                         in1=rdens[:, :].unsqueeze(2).to_broadcast([8, 4, 128]), op=ALU.mult)
                    ps, tp = next_ps()
                    psb = ps[:].bitcast(BF16)
                    for h in range(4):
                        S.op("pe", "transpose", [t_obs, t_idb], [tp], out=psb[:, h * TS:(h + 1) * TS], in_=obs[:, h, :],
                             identity=ident_bf[0:8, 0:8])
                    S.op("act", "activation", [tp], [t_obT[16]], out=obT[:, :, 2048 + i * TS:2048 + (i + 1) * TS],
                         in_=psb[:, 0:4 * TS].rearrange("p (h q) -> p h q", h=4), func=AF.Copy)
                    yield
            SGEN = gen_sample()

            def adv(k):
                for _ in range(k):
                    next(SGEN, None)

            for h in range(4):
                for g in range(4):
                    c0 = g * 512
                    n = 512
                    tsrc = [t_xnT[tt] for tt in range(g * 4, g * 4 + 4)]
                    ps, tp = next_ps()
                    for k in range(8):
                        S.op("pe", "matmul", tsrc + [t_wq], [tp], ps[:, 0:n], lhsT=wq[:, k, h * 128:(h + 1) * 128],
                             rhs=xnT[:, k, c0:c0 + n], start=(k == 0), stop=(k == 7))
                    S.op("act", "activation", [tp], [t_qT[g]], out=qT[:, c0:c0 + n], in_=ps[:, 0:n], func=AF.Copy)
                S.op("dve", "tensor_reduce", alltk, [t_km], out=km32[:, :],
                     in_=kT[:, h, 0:2048].rearrange("p (n k) -> p n k", n=8), axis=AX.X, op=ALU.add)
                S.op("dve", "tensor_copy", [t_km], [t_km], out=kmb[:], in_=km32[:])
                units = [(b, nb) for b in range(8) for nb in range(b + 1)]

                def QK(b, nb):
                    tq = t_qT[b // 2]
                    ps, tp = long_ps()
                    for j in range(2):
                        kc = nb * 256 + j * 128
                        S.op("pe", "matmul", [t_kT[2 * nb + j], tq], [tp], ps[:, j * 256:(j + 1) * 256],
                             lhsT=kT[:, h, kc:kc + 128], rhs=qT[:, b * 256:(b + 1) * 256], start=True, stop=True)
                    return ps, tp

                def GATE(b):
                    tq = t_qT[b // 2]
                    for qi in range(2):
                        ps, tp = next_ps()
                        qc = b * 256 + qi * 128
                        S.op("pe", "matmul", [tq, t_km], [tp], ps[:, 0:8], lhsT=qT[:, qc:qc + 128], rhs=kmb[:, :],
                             start=True, stop=True)
                        S.op("dve", "memset", [], [t_gs2], gsb[:], -1.0e30)
                        S.op("dve", "tensor_copy", [tp, t_gs2], [t_gs2], out=gsb[:, 0:b], in_=ps[:, 0:b])
                        S.op("dve", "max", [t_gs2], [t_gs2], out=m8[:], in_=gsb[:])
                        S.op("dve", "tensor_scalar", [t_gs2], [t_sel[qi]], out=selw[qi][:, 0:8], in0=gsb[:],
                             scalar1=m8[:, 2:3], scalar2=None, op0=ALU.is_ge)
                        S.op("dve", "tensor_scalar", [t_sel[qi], t_E], [t_sel[qi]], out=selw[qi][:, 8:16],
                             in0=selw[qi][:, 0:8], scalar1=cfar[:, h:h + 1], scalar2=None, op0=ALU.mult)

                def EXPPV(b, nb, ps, tp):
                    pi = cnt["pT"] % 2
                    cnt["pT"] += 1
                    if nb <= b - 2:
                        S.op("act", "activation", [tp], [t_pT[pi]], out=pT[pi][:], in_=ps[:, :], func=AF.Exp,
                             scale=SCALE)
                    else:
                        S.op("act", "activation", [tp], [t_pTf], out=pTf[:], in_=ps[:, :], func=AF.Exp, scale=SCALE)
                        Et = Eown if nb == b else Eadj
                        S.op("dve", "tensor_tensor", [t_pTf, t_E], [t_pT[pi]], out=pT[pi][:], in0=pTf[:],
                             in1=Et[:, h, :], op=ALU.mult)
                    po, tpo2 = next_ps()
                    for qi in range(2):
                        for j in range(2):
                            S.op("pe", "matmul", [t_pT[pi], t_vaug[2 * nb + j]], [tpo2],
                                 po[:, qi * 256:qi * 256 + 129],
                                 lhsT=pT[pi][:, j * 256 + qi * 128:j * 256 + qi * 128 + 128],
                                 rhs=vaug[:, 2 * nb + j, h, 0:129], start=(j == 0), stop=(j == 1))
                    for qi in range(2):
                        if nb == b:
                            w, rw = 1.0, []
                        elif nb == b - 1:
                            w, rw = (selw[qi][:, nb:nb + 1], [t_sel[qi]]) if b >= 4 else (1.0, [])
                        else:
                            w, rw = (selw[qi][:, 8 + nb:9 + nb], [t_sel[qi]]) if b >= 4 else (cfar[:, h:h + 1], [t_E])
                        src_ = po[:, qi * 256:qi * 256 + 129]
                        if nb == 0:
                            S.op("dve", "tensor_scalar", [tpo2] + rw, [t_acc[qi]], out=acc[qi][:, 0:129], in0=src_,
                                 scalar1=w, scalar2=None, op0=ALU.mult)
                        else:
                            S.op("dve", "scalar_tensor_tensor", [tpo2, t_acc[qi]] + rw, [t_acc[qi]],
                                 out=acc[qi][:, 0:129], in0=src_, scalar=w, in1=acc[qi][:, 0:129], op0=ALU.mult,
                                 op1=ALU.add)

                def FINAL(b):
                    for qi in range(2):
                        tt = 2 * b + qi
                        S.op("dve", "reciprocal", [t_acc[qi]], [t_obt], out=rden[:], in_=acc[qi][:, 128:129])
                        S.op("dve", "tensor_scalar", [t_acc[qi], t_obt], [t_obt], out=obt[:], in0=acc[qi][:, 0:128],
                             scalar1=rden[:, 0:1], scalar2=None, op0=ALU.mult)
                        ps, tp = next_ps()
                        psb = ps[:].bitcast(BF16)
                        S.op("pe", "transpose", [t_obt, t_idb], [tp], out=psb[:, 0:128], in_=obt[:], identity=ident_bf[:])
                        S.op("act", "activation", [tp], [t_obT[tt]], out=obT[:, h, tt * 128:(tt + 1) * 128],
                             in_=psb[:, 0:128], func=AF.Copy)

                pend = QK(*units[0])
                for u, (b, nb) in enumerate(units):
                    cur_qk = pend
                    if u + 1 < len(units):
                        pend = QK(*units[u + 1])
                    if nb == 0 and b >= 4:
                        GATE(b)
                    EXPPV(b, nb, *cur_qk)
                    if nb == b:
                        FINAL(b)
                    adv(3)

            for _ in SGEN:
                pass
            if DEBUG:
                S.dma("sp", t_obT, [], out=dbg_o[:, :], in_=obT[:, :, :].rearrange("p h n -> p (h n)"))
        S.barrier()
        esD.close()
        cur[0] = esM

        esE = ExitStack()
        cur[0] = esE
        t_mrgG = [Trk() for _ in range(5)]
        if STAGE >= 4:
            wg = sb("wg", [128, 8, 2048], BF16)
            t_wg = [Trk() for _ in range(4)]
            for c in range(4):
                for k in range(8):
                    S.dma("pool", [], [t_wg[c]], out=wg[:, k, c * 512:(c + 1) * 512],
                          in_=w_in[k * 128:(k + 1) * 128, 3584 + c * 512:3584 + (c + 1) * 512])
            wab = sb("wab", [128, 2, 4, 1024], BF16)
            t_wab = Trk()
            for c, wsrc in enumerate((wa_d, wb_d)):
                for k in range(4):
                    for hf in range(2):
                        S.dma("pool", [], [t_wab], out=wab[:, c, k, hf * 512:(hf + 1) * 512],
                              in_=wsrc[k * 128:(k + 1) * 128, hf * 512:(hf + 1) * 512])
            sga = sb("sga", [128, 512], F32)
            sgb = sb("sgb", [128, 512], F32)
            m1 = sb("m1", [128, 512], F32)
            m2 = sb("m2", [128, 512], F32)
            t_sga, t_sgb, t_m1, t_m2 = Trk(), Trk(), Trk(), Trk()
            gunits = [(j, g) for j in range(8) for g in range(5)]

            def GMM(u):
                j, g = gunits[u]
                c0 = g * 512
                n = 512 if g < 4 else 128
                tiles = list(range(g * 4, min(g * 4 + 4, NTILE)))
                tx = [t_xnT[tt] for tt in tiles]
                base = 4 * (u % 2)
                (psga, tpga), (psgb, tpgb), (psa, tpa), (psb_, tpb) = [(PS[base + q], t_ps[base + q]) for q in range(4)]
                for k in range(8):
                    S.op("pe", "matmul", tx + [t_wg[j // 4]], [tpga], psga[:, 0:n], lhsT=wg[:, k, j * 128:(j + 1) * 128],
                         rhs=xnT[:, k, c0:c0 + n], start=(k == 0), stop=(k == 7))
                for k in range(8):
                    S.op("pe", "matmul", tx + [t_wg[2 + j // 4]], [tpgb], psgb[:, 0:n],
                         lhsT=wg[:, k, 1024 + j * 128:1024 + (j + 1) * 128], rhs=xnT[:, k, c0:c0 + n],
                         start=(k == 0), stop=(k == 7))
                for k in range(4):
                    S.op("pe", "matmul", [t_oaT[k][g], t_wab], [tpa], psa[:, 0:n], lhsT=wab[:, 0, k, j * 128:(j + 1) * 128],
                         rhs=oaT[:, k, c0:c0 + n], start=(k == 0), stop=(k == 3))
                for k in range(4):
                    S.op("pe", "matmul", [t_obT[tt] for tt in tiles] + [t_wab], [tpb], psb_[:, 0:n],
                         lhsT=wab[:, 1, k, j * 128:(j + 1) * 128], rhs=obT[:, k, c0:c0 + n], start=(k == 0), stop=(k == 3))
                return (psga, tpga), (psgb, tpgb), (psa, tpa), (psb_, tpb)

            def GELEM(u, banks):
                j, g = gunits[u]
                c0 = g * 512
                n = 512 if g < 4 else 128
                (psga, tpga), (psgb, tpgb), (psa, tpa), (psb_, tpb) = banks
                S.op("act", "activation", [tpga], [t_sga], out=sga[:, 0:n], in_=psga[:, 0:n], func=AF.Sigmoid)
                S.op("act", "activation", [tpgb], [t_sgb], out=sgb[:, 0:n], in_=psgb[:, 0:n], func=AF.Sigmoid)
                S.op("dve", "tensor_tensor", [tpa, t_sga], [t_m1], out=m1[:, 0:n], in0=psa[:, 0:n], in1=sga[:, 0:n], op=ALU.mult)
                S.op("dve", "tensor_tensor", [tpb, t_sgb], [t_m2], out=m2[:, 0:n], in0=psb_[:, 0:n], in1=sgb[:, 0:n], op=ALU.mult)
                S.op("dve", "tensor_tensor", [t_m1, t_m2], [t_mrgG[g]], out=mergedT[:, j, c0:c0 + n], in0=m1[:, 0:n],
                     in1=m2[:, 0:n], op=ALU.add)

            gb_prev = GMM(0)
            for u in range(len(gunits)):
                gb_next = GMM(u + 1) if u + 1 < len(gunits) else None
                GELEM(u, gb_prev)
                gb_prev = gb_next
        S.barrier()
        esE.close()
        esM.close()
        cur[0] = es

        xres = sb("xres", [128, NTILE, D], F32)
        t_xr = [Trk() for _ in range(NTILE)]
        comb = sb("comb", [128, NTILE, 32], F32)
        t_comb = [Trk() for _ in range(NTILE)]
        junk2 = sb("junk2", [128, D], BF16)
        t_junk2 = Trk()
        xnb2 = [sb("xnb2_%d" % i, [128, D], BF16) for i in range(2)]
        t_xnb2 = [Trk() for _ in range(2)]
        gvec = sb("gvec", [128, 16], F32)
        t_gv = Trk()
        with nc.allow_non_contiguous_dma(reason="tiny gain vectors"):
            S.dma("sp", [], [t_gv], out=gvec[:, 0:8], in_=norm_xa_d.rearrange("o (k p) -> p (o k)", p=128))
            S.dma("sp", [], [t_gv], out=gvec[:, 8:16], in_=norm_ffn_d.rearrange("o (k p) -> p (o k)", p=128))

        wxq = sb("wxq", [128, 8, 512], BF16)
        wxo = sb("wxo", [128, 4, D], BF16)
        t_wxq, t_wxo = Trk(), Trk()
        wgu = [sb("wgu%d" % i, [128, 8, 512], BF16) for i in range(2)]
        wd = [sb("wd%d" % i, [128, 2, D], BF16) for i in range(2)]
        t_wgu = [Trk() for _ in range(2)]
        t_wd = [Trk() for _ in range(2)]

        stg, t_stg = [], []
        cnt["stg"] = 0

        def load_expert(e):
            bi = e % 2
            for piece in range(3):
                r = cnt["stg"] % 2
                cnt["stg"] += 1
                if piece < 2:
                    wsrc = (weg_d, weu_d)[piece]
                    S.dma("sp", [], [t_stg[r]], out=stg[r][:, :].rearrange("p (k n) -> p k n", k=8),
                          in_=wsrc[e].rearrange("(k p) n -> p k n", p=128))
                    S.op("pool", "tensor_copy", [t_stg[r]], [t_wgu[bi]], out=wgu[bi][:, :, piece * 256:(piece + 1) * 256],
                         in_=stg[r][:, :].rearrange("p (k n) -> p k n", k=8))
                else:
                    S.dma("sp", [], [t_stg[r]], out=stg[r][:, :].rearrange("p (k n) -> p k n", k=2),
                          in_=wed_d[e].rearrange("(k p) n -> p k n", p=128))
                    S.op("pool", "tensor_copy", [t_stg[r]], [t_wd[bi]], out=wd[bi][:, :, :],
                         in_=stg[r][:, :].rearrange("p (k n) -> p k n", k=2))

        def norm_S(tt, gain, dstT, dcol, t_dst):
            i = cnt["x"] % 2
            cnt["x"] += 1
            si = cnt["st"] % 32
            cnt["st"] += 1
            ts_ = t_stat[si]
            S.op("act", "activation", [t_xr[tt]], [t_junk2, ts_], out=junk2[:], in_=xres[:, tt, :], func=AF.Square,
                 accum_out=stat[:, si:si + 1])
            S.op("act", "activation", [ts_], [ts_], out=stat[:, 32 + si:33 + si], in_=stat[:, si:si + 1],
                 func=AF.Ln, scale=1.0 / D, bias=EPS)
            S.op("act", "activation", [ts_], [ts_], out=stat[:, 64 + si:65 + si], in_=stat[:, 32 + si:33 + si],
                 func=AF.Exp, scale=-0.5)
            S.op("dve", "tensor_scalar", [t_xr[tt], ts_], [t_xnb2[i]], out=xnb2[i][:], in0=xres[:, tt, :],
                 scalar1=stat[:, 64 + si:65 + si], scalar2=None, op0=ALU.mult)
            ps, tp = next_ps()
            psb = ps[:].bitcast(BF16)
            for k in range(8):
                S.op("pe", "transpose", [t_xnb2[i], t_idb], [tp], out=psb[:, k * 128:(k + 1) * 128],
                     in_=xnb2[i][:, k * 128:(k + 1) * 128], identity=ident_bf[:])
            S.op("dve", "tensor_tensor", [tp, t_gv], [t_dst], out=dstT[:, :, dcol:dcol + 128],
                 in0=psb.rearrange("p (k n) -> p k n", k=8),
                 in1=gain.unsqueeze(2).to_broadcast([128, 8, 128]), op=ALU.mult)
            return si

        esF = ExitStack()
        cur[0] = esF
        if STAGE >= 4:
            wmix = sb("wmix", [128, 8, D], BF16)
            t_wmix = Trk()
            for k in range(8):
                for hf in range(2):
                    S.dma("pool", [], [t_wmix], out=wmix[:, k, hf * 512:(hf + 1) * 512],
                          in_=wmix_d[k * 128:(k + 1) * 128, hf * 512:(hf + 1) * 512])
            for k in range(8):
                S.dma("pool", [], [t_wxq], out=wxq[:, k, :], in_=wxq_d[k * 128:(k + 1) * 128, :])
            for k in range(4):
                for hf in range(2):
                    S.dma("pool", [], [t_wxo], out=wxo[:, k, hf * 512:(hf + 1) * 512],
                          in_=wxo_d[k * 128:(k + 1) * 128, hf * 512:(hf + 1) * 512])
            for tt in range(NTILE):
                S.dma("sp", [], [t_xr[tt]], out=xres[:, tt, :], in_=xall[tt * 128:(tt + 1) * 128, :])
                for hf in range(2):
                    ps, tp = next_ps()
                    for k in range(8):
                        S.op("pe", "matmul", [t_mrgG[tt // 4], t_wmix], [tp], ps[:, :], lhsT=mergedT[:, k, tt * 128:(tt + 1) * 128],
                             rhs=wmix[:, k, hf * 512:(hf + 1) * 512], start=(k == 0), stop=(k == 7))
                    S.op("dve", "tensor_tensor", [tp, t_xr[tt]], [t_xr[tt]], out=xres[:, tt, hf * 512:(hf + 1) * 512],
                         in0=ps[:, :], in1=xres[:, tt, hf * 512:(hf + 1) * 512], op=ALU.add)
        S.barrier()
        esF.close()

        esG = ExitStack()
        cur[0] = esG
        if STAGE >= 5:
            SCALE = float(128 ** -0.5)
            x1nT = sb("x1nT", [128, 8, 512], BF16)
            t_x1n = [Trk() for _ in range(4)]
            xqT = sb("xqT", [128, 4, 512], BF16)
            t_xq = Trk()
            pTx = [sb("pTx%d" % j, [128, 512], BF16) for j in range(2)]
            t_pTx = [Trk() for _ in range(2)]
            xa = sb("xa", [128, 4, 4, 128], BF16)
            t_xa = [Trk() for _ in range(4)]
            rdx = sb("rdx", [128, 4], F32)
            t_rdx = Trk()
            xaT = sb("xaT", [128, 4, 512], BF16)
            t_xaT = [Trk() for _ in range(4)]
            mst = [sb("mst%d" % i, [128, 512], F32) for i in range(4)]
            t_mst = [Trk() for _ in range(4)]
            mkTs = sb("mkTs", [128, 4, 256], BF16)
            t_mkTs = Trk()
            mvs = sb("mvs", [128, 2, 4, 132], BF16)
            t_mvs = Trk()
            S.op("pool", "memset", [], [t_mvs], mvs[:, :, :, 128:132], 1.0)
            pTxs = sb("pTxs", [128, 64], BF16)
            t_pTxs = Trk()
            xas = sb("xas", [8, 4, 128], BF16)
            rdxs = sb("rdxs", [8, 4], F32)
            t_xas = Trk()
            for g in range(5):
                n = 512 if g < 4 else 128
                tiles = list(range(g * 4, min(g * 4 + 4, NTILE)))
                for ti, tt in enumerate(tiles):
                    norm_S(tt, gvec[:, 0:8], x1nT, ti * 128, t_x1n[ti])
                tx = [t_x1n[ti] for ti in range(len(tiles))]
                for h in range(4):
                    ps, tp = next_ps()
                    for k in range(8):
                        S.op("pe", "matmul", tx + [t_wxq], [tp], ps[:, 0:n], lhsT=wxq[:, k, h * 128:(h + 1) * 128],
                             rhs=x1nT[:, k, 0:n], start=(k == 0), stop=(k == 7))
                    S.op("act", "activation", [tp], [t_xq], out=xqT[:, h, 0:n], in_=ps[:, 0:n], func=AF.Copy)
                if g < 4:
                    for h in range(4):
                        for j in range(2):
                            ps, tp = next_ps()
                            S.op("pe", "matmul", [t_memkT[j], t_xq], [tp], ps[:, 0:n], lhsT=memkT[:, h, j * 128:(j + 1) * 128],
                                 rhs=xqT[:, h, 0:n], start=True, stop=True)
                            S.op("act", "activation", [tp], [t_pTx[j]], out=pTx[j][:, 0:n], in_=ps[:, 0:n], func=AF.Exp,
                                 scale=SCALE)
                        for ti in range(4):
                            po, tpo2 = next_ps()
                            for j in range(2):
                                S.op("pe", "matmul", [t_pTx[j], t_memvaug[j]], [tpo2], po[:, 0:129],
                                     lhsT=pTx[j][:, ti * 128:(ti + 1) * 128], rhs=memvaug[:, j, h, 0:129],
                                     start=(j == 0), stop=(j == 1))
                            S.op("dve", "reciprocal", [tpo2], [t_rdx], out=rdx[:, 0:1], in_=po[:, 128:129])
                            S.op("dve", "tensor_scalar", [tpo2, t_rdx], [t_xa[ti]], out=xa[:, ti, h, :], in0=po[:, 0:128],
                                 scalar1=rdx[:, 0:1], scalar2=None, op0=ALU.mult)
                    for ti in range(4):
                        ps, tp = next_ps()
                        psb = ps[:].bitcast(BF16)
                        for h in range(4):
                            S.op("pe", "transpose", [t_xa[ti], t_idb], [tp], out=psb[:, h * 128:(h + 1) * 128],
                                 in_=xa[:, ti, h, :], identity=ident_bf[:])
                        S.op("act", "activation", [tp], [t_xaT[ti]], out=xaT[:, :, ti * 128:(ti + 1) * 128],
                             in_=psb[:, 0:512].rearrange("p (h n) -> p h n", h=4), func=AF.Copy)
                else:
                    def mload(i):
                        o = 0
                        for j in range(2):
                            S.dma("sp", [], [t_mst[o + j]], out=mst[o + j][:], in_=cmk_d[i * 256 + j * 128:i * 256 + (j + 1) * 128, :])
                            S.dma("sp", [], [t_mst[o + 2 + j]], out=mst[o + 2 + j][:],
                                  in_=cmv_d[i * 256 + j * 128:i * 256 + (j + 1) * 128, :])
                    for i in range(NS):
                        mload(i)
                        o = 0
                        for j in range(2):
                            ps, tp = next_ps()
                            for h in range(4):
                                S.op("pe", "transpose", [t_mst[o + j], t_cst], [tp], out=ps[:, h * 128:(h + 1) * 128],
                                     in_=mst[o + j][:, h * 128:(h + 1) * 128], identity=cst[:, 0:128])
                            S.op("act", "activation", [tp], [t_mkTs], out=mkTs[:, :, j * 128:(j + 1) * 128],
                                 in_=ps[:, :].rearrange("p (h n) -> p h n", h=4), func=AF.Copy)
                            S.op("dve", "tensor_copy", [t_mst[o + 2 + j]], [t_mvs], out=mvs[:, j, :, 0:128],
                                 in_=mst[o + 2 + j][:, :].rearrange("p (h d) -> p h d", h=4))
                        ps, tp = next_ps()
                        for j in range(2):
                            for h in range(4):
                                cc = (j * 4 + h) * TS
                                S.op("pe", "matmul", [t_mkTs, t_xq], [tp], ps[:, cc:cc + TS], lhsT=mkTs[:, h, j * 128:(j + 1) * 128],
                                     rhs=xqT[:, h, i * TS:(i + 1) * TS], start=True, stop=True)
                        S.op("act", "activation", [tp], [t_pTxs], out=pTxs[:, :], in_=ps[:, 0:64], func=AF.Exp, scale=SCALE)
                        for h in range(4):
                            po, tpo2 = next_ps()
                            for j in range(2):
                                cc = (j * 4 + h) * TS
                                S.op("pe", "matmul", [t_pTxs, t_mvs], [tpo2], po[0:8, 0:129], lhsT=pTxs[:, cc:cc + TS],
                                     rhs=mvs[:, j, h, 0:129], start=(j == 0), stop=(j == 1))
                            S.op("dve", "reciprocal", [tpo2], [t_xas], out=rdxs[:, h:h + 1], in_=po[0:8, 128:129])
                            S.op("dve", "tensor_scalar", [tpo2, t_xas], [t_xas], out=xas[:, h, :], in0=po[0:8, 0:128],
                                 scalar1=rdxs[:, h:h + 1], scalar2=None, op0=ALU.mult)
                        ps, tp = next_ps()
                        psb = ps[:].bitcast(BF16)
                        for h in range(4):
                            S.op("pe", "transpose", [t_xas, t_idb], [tp], out=psb[:, h * TS:(h + 1) * TS], in_=xas[:, h, :],
                                 identity=ident_bf[0:8, 0:8])
                        S.op("act", "activation", [tp], [t_xaT[0]], out=xaT[:, :, i * TS:(i + 1) * TS],
                             in_=psb[:, 0:4 * TS].rearrange("p (h q) -> p h q", h=4), func=AF.Copy)
                for ti, tt in enumerate(tiles):
                    for hf in range(2):
                        ps, tp = next_ps()
                        for h in range(4):
                            S.op("pe", "matmul", [t_xaT[ti], t_wxo], [tp], ps[:, :], lhsT=xaT[:, h, ti * 128:(ti + 1) * 128],
                                 rhs=wxo[:, h, hf * 512:(hf + 1) * 512], start=(h == 0), stop=(h == 3))
                        S.op("dve", "tensor_tensor", [tp, t_xr[tt]], [t_xr[tt]], out=xres[:, tt, hf * 512:(hf + 1) * 512],
                             in0=ps[:, :], in1=xres[:, tt, hf * 512:(hf + 1) * 512], op=ALU.add)
        S.barrier()
        esG.close()

        esH = ExitStack()
        cur[0] = esH
        xfT = mergedT
        t_xf = [Trk() for _ in range(NTILE)]
        if STAGE >= 6:
            wr = sb("wr", [128, 8, 36], BF16)
            t_wr = Trk()
            with nc.allow_non_contiguous_dma(reason="narrow router weights"):
                for k in range(8):
                    S.dma("pool", [], [t_wr], out=wr[:, k, 0:4], in_=wrg_d[k * 128:(k + 1) * 128, :])
                    S.dma("pool", [], [t_wr], out=wr[:, k, 4:36], in_=wre_d[k * 128:(k + 1) * 128, :])
            rb = sb("rb", [128, 36], F32)
            t_rb = Trk()
            with nc.allow_non_contiguous_dma(reason="bias broadcast"):
                S.dma("sp", [], [t_rb], out=rb[:, 0:4], in_=bass.AP(brg_d.tensor, 0, [[0, 128], [1, 4]]))
                S.dma("sp", [], [t_rb], out=rb[:, 4:36], in_=bass.AP(bre_d.tensor, 0, [[0, 128], [1, 32]]))
            lg = sb("lg", [128, 36], F32)
            rt = sb("rt", [128, 64], F32)
            t_rt = Trk()
            for tt in range(NTILE):
                norm_S(tt, gvec[:, 8:16], xfT, tt * 128, t_xf[tt])
                ps, tp = next_ps()
                for k in range(8):
                    S.op("pe", "matmul", [t_xf[tt], t_wr], [tp], ps[:, 0:36], lhsT=xfT[:, k, tt * 128:(tt + 1) * 128],
                         rhs=wr[:, k, :], start=(k == 0), stop=(k == 7))
                R_, W_ = [t_rt], [t_rt]
                S.op("dve", "tensor_tensor", [tp, t_rb], W_, out=lg[:], in0=ps[:, 0:36], in1=rb[:], op=ALU.add)
                S.op("dve", "tensor_reduce", R_, W_, out=rt[:, 0:1], in_=lg[:, 0:4], axis=AX.X, op=ALU.max)
                S.op("dve", "tensor_scalar", R_, W_, out=rt[:, 1:2], in0=rt[:, 0:1], scalar1=-1.0, scalar2=None, op0=ALU.mult)
                S.op("dve", "tensor_scalar", R_, W_, out=rt[:, 4:8], in0=lg[:, 0:4], scalar1=rt[:, 0:1], scalar2=None,
                     op0=ALU.is_equal)
                S.op("act", "activation", R_, W_, out=rt[:, 8:12], in_=lg[:, 0:4], func=AF.Exp, bias=rt[:, 1:2],
                     accum_out=rt[:, 2:3])
                S.op("dve", "reciprocal", R_, W_, out=rt[:, 3:4], in_=rt[:, 2:3])
                S.op("dve", "tensor_scalar", R_, W_, out=rt[:, 16:24], in0=lg[:, 4:12], scalar1=rt[:, 4:5], scalar2=None,
                     op0=ALU.mult)
                for gi in range(1, 4):
                    S.op("dve", "scalar_tensor_tensor", R_, W_, out=rt[:, 16:24], in0=lg[:, 4 + gi * 8:12 + gi * 8],
                         scalar=rt[:, 4 + gi:5 + gi], in1=rt[:, 16:24], op0=ALU.mult, op1=ALU.add)
                S.op("dve", "max", R_, W_, out=rt[:, 24:32], in_=rt[:, 16:24])
                S.op("dve", "tensor_tensor", R_, W_, out=rt[:, 32:33], in0=rt[:, 25:26], in1=rt[:, 24:25], op=ALU.subtract)
                S.op("act", "activation", R_, W_, out=rt[:, 33:34], in_=rt[:, 32:33], func=AF.Sigmoid)
                S.op("dve", "tensor_scalar", R_, W_, out=rt[:, 34:35], in0=rt[:, 33:34], scalar1=-1.0, scalar2=1.0,
                     op0=ALU.mult, op1=ALU.add)
                S.op("dve", "tensor_scalar", R_, W_, out=rt[:, 34:35], in0=rt[:, 34:35], scalar1=rt[:, 3:4], scalar2=None,
                     op0=ALU.mult)
                S.op("dve", "tensor_scalar", R_, W_, out=rt[:, 33:34], in0=rt[:, 33:34], scalar1=rt[:, 3:4], scalar2=None,
                     op0=ALU.mult)
                S.op("dve", "tensor_scalar", R_, W_, out=rt[:, 40:48], in0=rt[:, 16:24], scalar1=rt[:, 24:25],
                     scalar2=rt[:, 34:35], op0=ALU.is_equal, op1=ALU.mult)
                S.op("dve", "tensor_scalar", R_, W_, out=rt[:, 48:56], in0=rt[:, 16:24], scalar1=rt[:, 25:26],
                     scalar2=rt[:, 33:34], op0=ALU.is_equal, op1=ALU.mult)
                S.op("dve", "tensor_tensor", R_, W_, out=rt[:, 40:48], in0=rt[:, 40:48], in1=rt[:, 48:56], op=ALU.add)
                for gi in range(4):
                    S.op("dve", "tensor_scalar", R_, [t_comb[tt]], out=comb[:, tt, gi * 8:(gi + 1) * 8], in0=rt[:, 40:48],
                         scalar1=rt[:, 4 + gi:5 + gi], scalar2=None, op0=ALU.mult)
            sgl = [sb("sgl%d" % i, [128, 512], F32) for i in range(2)]
            t_sgl = [Trk() for _ in range(2)]
            hT = [sb("hT%d" % i, [128, 2, 512], BF16) for i in range(2)]
            t_hT = [[Trk() for _ in range(2)] for _ in range(2)]
            cnt["gu"] = 0
            cnt["dn"] = 0

            def gu_ps():
                i = 2 + cnt["gu"] % 4
                cnt["gu"] += 1
                return PS[i], t_ps[i]

            def dn_ps():
                i = 6 + cnt["dn"] % 2
                cnt["dn"] += 1
                return PS[i], t_ps[i]

            def geom(g):
                return g * 512, (512 if g < 4 else 128), list(range(g * 4, min(g * 4 + 4, NTILE)))

            def GU(e, g):
                bi = e % 2
                c0, n, tiles = geom(g)
                tx = [t_xf[tt] for tt in tiles]
                pgu = []
                for c in range(4):
                    ps, tp = gu_ps()
                    for k in range(8):
                        S.op("pe", "matmul", tx + [t_wgu[bi]], [tp], ps[:, 0:n], lhsT=wgu[bi][:, k, c * 128:(c + 1) * 128],
                             rhs=xfT[:, k, c0:c0 + n], start=(k == 0), stop=(k == 7))
                    pgu.append((ps, tp))
                return pgu

            def ELEM(step, g, pgu):
                c0, n, tiles = geom(g)
                hb = step % 2
                for fh in range(2):
                    S.op("act", "activation", [pgu[fh][1]], [t_sgl[fh]], out=sgl[fh][:, 0:n], in_=pgu[fh][0][:, 0:n],
                         func=AF.Silu)
                    S.op("dve", "tensor_tensor", [pgu[2 + fh][1], t_sgl[fh]], [t_hT[hb][fh]], out=hT[hb][:, fh, 0:n],
                         in0=pgu[2 + fh][0][:, 0:n], in1=sgl[fh][:, 0:n], op=ALU.mult)

            def DOWN(step, e, g):
                bi = e % 2
                hb = step % 2
                c0, n, tiles = geom(g)
                for ti, tt in enumerate(tiles):
                    for hf in range(2):
                        ps, tp = dn_ps()
                        for fh in range(2):
                            S.op("pe", "matmul", [t_hT[hb][fh], t_wd[bi]], [tp], ps[:, :], lhsT=hT[hb][:, fh, ti * 128:(ti + 1) * 128],
                                 rhs=wd[bi][:, fh, hf * 512:(hf + 1) * 512], start=(fh == 0), stop=(fh == 1))
                        S.op("dve", "scalar_tensor_tensor", [tp, t_comb[tt], t_xr[tt]], [t_xr[tt]],
                             out=xres[:, tt, hf * 512:(hf + 1) * 512], in0=ps[:, :], scalar=comb[:, tt, e:e + 1],
                             in1=xres[:, tt, hf * 512:(hf + 1) * 512], op0=ALU.mult, op1=ALU.add)

            for i_ in range(2):
                stg.append(sb("stg%d" % i_, [128, 2048], F32))
                t_stg.append(Trk())
            load_expert(0)
            steps = [(e, g) for e in range(32) for g in range(5)]
            prev = None
            for si_, (e, g) in enumerate(steps):
                pgu = GU(e, g)
                if prev is not None:
                    DOWN(si_ - 1, *prev)
                if g == 0 and e + 1 < 32:
                    load_expert(e + 1)
                ELEM(si_, g, pgu)
                prev = (e, g)
            DOWN(len(steps) - 1, *prev)
        gfin = sb("gfin", [128, D], F32)
        t_gfin = Trk()
        with nc.allow_non_contiguous_dma(reason="gain broadcast"):
            S.dma("sp", [], [t_gfin], out=gfin[:], in_=bass.AP(nfin_d.tensor, 0, [[0, 128], [1, D]]))
        yt = [sb("yt%d" % i, [128, D], F32) for i in range(2)]
        t_yt = [Trk() for _ in range(2)]
        for tt in range(NTILE):
            si = cnt["st"] % 32
            cnt["st"] += 1
            ts_ = t_stat[si]
            S.op("act", "activation", [t_xr[tt]], [t_junk2, ts_], out=junk2[:], in_=xres[:, tt, :], func=AF.Square,
                 accum_out=stat[:, si:si + 1])
            S.op("act", "activation", [ts_], [ts_], out=stat[:, 32 + si:33 + si], in_=stat[:, si:si + 1],
                 func=AF.Ln, scale=1.0 / D, bias=EPS)
            S.op("act", "activation", [ts_], [ts_], out=stat[:, 64 + si:65 + si], in_=stat[:, 32 + si:33 + si],
                 func=AF.Exp, scale=-0.5)
            yi = tt % 2
            S.op("dve", "scalar_tensor_tensor", [t_xr[tt], ts_, t_gfin], [t_yt[yi]], out=yt[yi][:], in0=xres[:, tt, :],
                 scalar=stat[:, 64 + si:65 + si], in1=gfin[:], op0=ALU.mult, op1=ALU.mult)
            S.dma("sp", [t_yt[yi]], [], out=y_o[tt * 128:(tt + 1) * 128, :], in_=yt[yi][:])
        S.barrier()
        esH.close()
        cur[0] = es

        S.finish()
    return nc


_CACHE = {}


def make_ckv(I):
    npool = I["cache_k"].shape[1]
    return np.concatenate([np.asarray(I["cache_k"])[0].reshape(npool * 128, 512),
                           np.asarray(I["cache_v"])[0].reshape(npool * 128, 512)], axis=1)


def make_in_map(c, I):
    f = lambda a: np.ascontiguousarray(np.asarray(a))
    xall = np.concatenate([f(I["x_prompt"])[c], f(I["x_sample"])[c * NS:(c + 1) * NS].reshape(NS * TS, D)], axis=0)
    npool = I["cache_k"].shape[1]
    return {
        "xall": xall,
        "memp": f(I["mem_prompt"])[c],
        "consts": host_consts(),
        "consts2": host_consts2(),
        "norm_mix": f(I["norm_mix"]), "norm_mem": f(I["norm_mem"]),
        "w_in": f(I["w_in"])[0], "w_xk": f(I["w_xk"])[0], "w_xv": f(I["w_xv"])[0],
        "lbl": f(I["hg_lb_logits"]), "hg_norm": f(I["hg_norm"]),
        "s0": f(I["state_hgrn"])[0, c * NS:(c + 1) * NS].reshape(NS * 4, 128, 128),
        "rel_table": f(I["rel_table"]),
        "cache_kv": I["_ckv"],
        "page_table": f(I["page_table"])[c * NS:(c + 1) * NS],
        "w_branch_a": f(I["w_branch_a"])[0], "w_branch_b": f(I["w_branch_b"])[0], "w_mix_out": f(I["w_mix_out"])[0],
        "norm_xattn": f(I["norm_xattn"]), "w_xq": f(I["w_xq"])[0], "w_xo": f(I["w_xo"])[0],
        "cache_mem_k": f(I["cache_mem_k"])[0, c * NS:(c + 1) * NS].reshape(NS * 256, 512),
        "cache_mem_v": f(I["cache_mem_v"])[0, c * NS:(c + 1) * NS].reshape(NS * 256, 512),
        "norm_ffn": f(I["norm_ffn"]), "w_group_router": f(I["w_group_router"])[0],
        "w_expert_router": f(I["w_expert_router"])[0], "b_group_router": f(I["b_group_router"]),
        "b_expert_router": f(I["b_expert_router"]),
        "w_expert_gate": f(I["w_expert_gate"])[0].reshape(32, D, 256),
        "w_expert_up": f(I["w_expert_up"])[0].reshape(32, D, 256),
        "w_expert_down": f(I["w_expert_down"])[0].reshape(32, 256, D),
        "norm_final": f(I["norm_final"]).reshape(1, D),
    }


def kernel(**I):
    npool = I["cache_k"].shape[1]
    key = ("nc", npool)
    if key not in _CACHE:
        _CACHE[key] = build_nc(npool)
    nc = _CACHE[key]
    I = dict(I)
    I["_ckv"] = make_ckv(I)
    in_maps = [make_in_map(c, I) for c in range(NCORES)]
    res = run_bass_kernel_spmd(nc, in_maps, core_ids=list(range(NCORES)))
    R = res.results
    cat = lambda name, sl: np.stack([R[c][name][sl] for c in range(NCORES)])
    y_prompt = cat("y", slice(0, T))
    y_sample = cat("y", slice(T, NT)).reshape(128, TS, D)
    k_prompt = cat("kout", slice(0, T)).reshape(1, 8, T, 4, 128)
    v_prompt = cat("vout", slice(0, T)).reshape(1, 8, T, 4, 128)
    k_sample = cat("kout", slice(T, NT)).reshape(1, 128, TS, 4, 128)
    v_sample = cat("vout", slice(T, NT)).reshape(1, 128, TS, 4, 128)
    sp = cat("spout", slice(None)).reshape(1, 8, 4, 128, 128)
    ss = cat("ssout", slice(None)).reshape(1, 128, 4, 128, 128)
    mk = cat("mkout", slice(None)).reshape(1, 8, 256, 4, 128)
    mv = cat("mvout", slice(None)).reshape(1, 8, 256, 4, 128)
    return (y_prompt, y_sample, k_prompt, v_prompt, sp, mk, mv, k_sample, v_sample, ss)
```

```python
import os
import numpy as np
from contextlib import ExitStack
import concourse.bass as bass
import concourse.mybir as mybir
from concourse.bass_utils import run_bass_kernel_spmd

F32 = mybir.dt.float32
BF16 = mybir.dt.bfloat16
I32 = mybir.dt.int32
AF = mybir.ActivationFunctionType
ALU = mybir.AluOpType
AX = mybir.AxisListType

NCORES = 8
D = 1024
T = 2048
NS = 16
TS = 8
NT = T + NS * TS
NTILE = NT // 128
EPS = 1e-6
STAGE = int(os.environ.get('KSTAGE', '9'))
DEBUG = os.environ.get('KDEBUG') == '1'
CW = 1536
LIMIT = int(os.environ.get('KLIMIT', '100000000'))


class Trk:
    __slots__ = ("w", "r", "excl")

    def __init__(self, excl=False):
        self.w = {}
        self.r = {}
        self.excl = excl


class Eng:
    def __init__(self, name, obj, sem):
        self.name, self.o, self.sem = name, obj, sem
        self.cnt = 0
        self.seen = {}

    def wait(self, tok):
        key, sem, val = tok
        if self.seen.get(key, 0) < val:
            self.o.wait_ge(sem, val)
            self.seen[key] = val


class Sched:
    def __init__(self, nc, es):
        self.nc = nc
        self.E = {}
        for name, obj in (("pe", nc.tensor), ("act", nc.scalar), ("dve", nc.vector),
                          ("pool", nc.gpsimd), ("sp", nc.sync)):
            self.E[name] = Eng(name, obj, es.enter_context(nc.semaphore("sem_" + name)))
        self.dsl = {}
        for q, n in (("sp", 24), ("pool", 16), ("act", 8)):
            self.dsl[q] = [[("d%s%d" % (q, i)), es.enter_context(nc.semaphore("d%s%d" % (q, i))), 0]
                           for i in range(n)]
        self.dpos = {"sp": 0, "pool": 0, "act": 0}
        self.nops = 0

    def _deps(self, e, R, W):
        for t in R:
            for tok in t.w.values():
                e.wait(tok)
            if t.excl:
                for tok in t.r.values():
                    if tok[0] != e.name:
                        e.wait(tok)
        for t in W:
            for tok in t.w.values():
                if e.name == "pe" and tok[0] == "pe":
                    continue
                e.wait(tok)
            for tok in t.r.values():
                if e.name == "pe" and tok[0] == "pe":
                    continue
                e.wait(tok)

    def _mark(self, tok, R, W):
        for t in R:
            old = t.r.get(tok[0])
            if old is None or old[2] < tok[2]:
                t.r[tok[0]] = tok
        for t in W:
            t.w = {tok[0]: tok}
            t.r = {}

    def op(self, eng, meth, R, W, *a, **k):
        self.nops += 1
        if self.nops > LIMIT:
            return None
        e = self.E[eng]
        self._deps(e, R, W)
        inst = getattr(e.o, meth)(*a, **k)
        e.cnt += 1
        inst.then_inc(e.sem, 1)
        self._mark((e.name, e.sem, e.cnt), R, W)
        return inst

    def dma(self, q, R, W, meth="dma_start", **k):
        self.nops += 1
        if self.nops > LIMIT:
            return None
        e = self.E[q]
        self._deps(e, R, W)
        slots = self.dsl[q]
        s = slots[self.dpos[q] % len(slots)]
        self.dpos[q] += 1
        e.wait((s[0], s[1], s[2]))
        inst = getattr(e.o, meth)(**k)
        s[2] += 16
        inst.then_inc(s[1], 16)
        self._mark((s[0], s[1], s[2]), R, W)

    def barrier(self):
        toks = [(e.name, e.sem, e.cnt) for e in self.E.values() if e.cnt > 0]
        for q, slots in self.dsl.items():
            for s in slots:
                if s[2] > 0:
                    toks.append((s[0], s[1], s[2]))
        for e in self.E.values():
            for tok in toks:
                e.wait(tok)

    def finish(self):
        for q, slots in self.dsl.items():
            e = self.E[q]
            for s in slots:
                if s[2] > 0:
                    e.wait((s[0], s[1], s[2]))


def host_consts():
    c = np.zeros((128, CW), np.float32)
    c[:, 0:128] = np.eye(128, dtype=np.float32)
    p = np.arange(128)[:, None]
    j = np.arange(128)[None, :]
    c[:, 128:256] = (j >= p).astype(np.float32)
    c[:, 256:384] = ((j >= p) & (j // TS == p // TS)).astype(np.float32)
    c[:, 384:512] = ((j >= p) & (j // 64 == p // 64)).astype(np.float32)
    c[:, 512:1024] = (np.arange(512) % 64 != 0).astype(np.float32)[None, :]
    c[:, 1024:1152] = (np.arange(128) % TS != 0).astype(np.float32)[None, :]
    c[:, 1152:1280] = 1.0 / 128.0
    for jj in range(8):
        c[:, 1280 + jj] = ((np.arange(128) // TS) % 8 == jj).astype(np.float32)
    c[:, 1300] = np.arange(128)
    c[:, 1320:1448] = np.eye(128, dtype=np.float32)[::-1]
    return c


def rel_bucket_np(rel):
    d = np.maximum(rel, 0)
    df = np.maximum(d, 1).astype(np.float32)
    large = 16 + (np.log(df / np.float32(16)) / np.float32(np.log(128 / 16)) * np.float32(16)).astype(np.int32)
    large = np.minimum(large, 31)
    return np.where(d < 16, d, large)


def host_consts2():
    rel = np.arange(768) - 256
    bk = rel_bucket_np(rel)
    c = np.zeros((36, 768), np.float32)
    c[bk, np.arange(768)] = 1.0
    c[0:32, rel < 0] = 0.0
    c[32:36, :] = (rel >= 0).astype(np.float32)[None, :]
    return c


def build_nc(npool=2560):
    nc = bass.Bass("TRN2", target_bir_lowering=False)

    def din(name, shape, dt=F32):
        return nc.dram_tensor(name, list(shape), dt, kind="ExternalInput").ap()

    def dout(name, shape, dt=F32):
        return nc.dram_tensor(name, list(shape), dt, kind="ExternalOutput").ap()

    xall = din("xall", [NT, D])
    memp = din("memp", [256, D])
    consts = din("consts", [128, CW])
    norm_mix = din("norm_mix", [1, D])
    norm_mem = din("norm_mem", [1, D])
    w_in = din("w_in", [D, 5632])
    w_xk = din("w_xk", [D, 512])
    w_xv = din("w_xv", [D, 512])
    lbl = din("lbl", [2, 512])
    hgn_d = din("hg_norm", [1, 512])
    s0_d = din("s0", [NS * 4, 128, 128])
    consts2 = din("consts2", [36, 768])
    rel_d = din("rel_table", [32, 4])
    ckv_d = din("cache_kv", [npool * 128, 1024])
    pt_d = din("page_table", [NS, 16], I32)
    wa_d = din("w_branch_a", [512, D])
    wb_d = din("w_branch_b", [512, D])
    wmix_d = din("w_mix_out", [D, D])
    norm_xa_d = din("norm_xattn", [1, D])
    wxq_d = din("w_xq", [D, 512])
    wxo_d = din("w_xo", [512, D])
    cmk_d = din("cache_mem_k", [NS * 256, 512])
    cmv_d = din("cache_mem_v", [NS * 256, 512])
    norm_ffn_d = din("norm_ffn", [1, D])
    wrg_d = din("w_group_router", [D, 4])
    wre_d = din("w_expert_router", [D, 32])
    brg_d = din("b_group_router", [1, 4])
    bre_d = din("b_expert_router", [1, 32])
    weg_d = din("w_expert_gate", [32, D, 256])
    weu_d = din("w_expert_up", [32, D, 256])
    wed_d = din("w_expert_down", [32, 256, D])
    nfin_d = din("norm_final", [1, D])
    et_scr_t = nc.dram_tensor("et_scr", [4, 768], F32, kind="Internal")
    et_scr = et_scr_t.ap()

    y_o = dout("y", [NT, D])
    k_o = dout("kout", [NT, 512])
    v_o = dout("vout", [NT, 512])
    mk_o = dout("mkout", [256, 512])
    mv_o = dout("mvout", [256, 512])
    sp_o = dout("spout", [4, 128, 128])
    ss_o = dout("ssout", [NS * 4, 128, 128])
    dbg_o = dout("dbg", [128, 4 * NT], BF16) if DEBUG else None

    with ExitStack() as es:
        S = Sched(nc, es)

        cur = [es]

        def sb(name, shape, dt):
            try:
                return cur[0].enter_context(nc.sbuf_tensor(name, list(shape), dt))
            finally:
                if os.environ.get("KALLOC"):
                    nb = int(np.prod(shape[1:])) * (2 if dt == BF16 else 4)
                    print("ALLOC", name, shape, nb)

        cst = sb("cst", [128, CW], F32)
        t_cst = Trk()
        S.dma("sp", [], [t_cst], out=cst[:], in_=consts[:, :])
        ident_bf = sb("ident_bf", [128, 128], BF16)
        t_idb = Trk()
        S.op("dve", "tensor_copy", [t_cst], [t_idb], out=ident_bf[:], in_=cst[:, 0:128])
        g_mix = sb("g_mix", [128, 8], F32)
        g_mem = sb("g_mem", [128, 8], F32)
        t_g = Trk()
        with nc.allow_non_contiguous_dma(reason="tiny gain vectors"):
            S.dma("sp", [], [t_g], out=g_mix[:], in_=norm_mix.rearrange("o (k p) -> p (o k)", p=128))
            S.dma("sp", [], [t_g], out=g_mem[:], in_=norm_mem.rearrange("o (k p) -> p (o k)", p=128))

        PS = [es.enter_context(nc.psum_tensor("ps%d" % i, [128, 512], F32)) for i in range(8)]
        t_ps = [Trk(excl=True) for _ in range(8)]

        memkT = sb("memkT", [128, 4, 256], BF16)
        t_memkT = [Trk() for _ in range(2)]
        memvaug = sb("memvaug", [128, 2, 4, 132], BF16)
        t_memvaug = [Trk() for _ in range(2)]
        stat = sb("stat", [128, 3 * 32], F32)
        t_stat = [Trk() for _ in range(32)]
        RA = sb("RA", [128, 4 * NT + NTILE * 4 * 132], BF16)
        kT = RA[:, 0:4 * NT].rearrange("p (h n) -> p h n", h=4)
        vaug = RA[:, 4 * NT:4 * NT + NTILE * 4 * 132].rearrange("p (t h d) -> p t h d", t=NTILE, h=4)
        mergedT = RA[:, 0:8 * NT].rearrange("p (k n) -> p k n", k=8)
        t_mrg = [Trk() for _ in range(NTILE)]
        t_kT = [Trk() for _ in range(NTILE)]
        t_vaug = [Trk() for _ in range(NTILE)]
        esM = ExitStack()
        cur[0] = esM
        xnT = sb("xnT", [128, 8, NT], BF16)
        t_xnT = [Trk() for _ in range(NTILE)]
        oaT = sb("oaT", [128, 4, NT], BF16)
        t_oaT = [[Trk() for _ in range(5)] for _ in range(4)]
        obT = sb("obT", [128, 4, NT], BF16)
        t_obT = [Trk() for _ in range(NTILE)]
        esA = ExitStack()
        cur[0] = esA
        vhg = sb("vhg", [128, NTILE, 512], BF16)
        t_vhg = [Trk() for _ in range(NTILE)]
        esB = ExitStack()
        cur[0] = esB
        memnT = sb("memnT", [128, 8, 256], BF16)
        t_memnT = [Trk() for _ in range(2)]
        xt = [sb("xt%d" % i, [128, D], F32) for i in range(2)]
        t_xt = [Trk() for _ in range(2)]
        junk = sb("junk", [128, D], BF16)
        t_junk = Trk()
        xnb = [sb("xnb%d" % i, [128, D], BF16) for i in range(2)]
        t_xnb = [Trk() for _ in range(2)]

        cnt = {"ps": 0, "x": 0, "st": 0, "lps": 0}

        def next_ps():
            i = 2 + cnt["ps"] % 6
            cnt["ps"] += 1
            return PS[i], t_ps[i]

        def long_ps():
            i = cnt["lps"] % 2
            cnt["lps"] += 1
            return PS[i], t_ps[i]

        def norm_T(src_rows, gain, dstT, dcol, t_dst):
            i = cnt["x"] % 2
            cnt["x"] += 1
            si = cnt["st"] % 32
            cnt["st"] += 1
            S.dma("sp", [], [t_xt[i]], out=xt[i][:], in_=src_rows)
            ts_ = t_stat[si]
            S.op("act", "activation", [t_xt[i]], [t_junk, ts_], out=junk[:], in_=xt[i][:], func=AF.Square,
                 accum_out=stat[:, si:si + 1])
            S.op("act", "activation", [ts_], [ts_], out=stat[:, 32 + si:33 + si], in_=stat[:, si:si + 1],
                 func=AF.Ln, scale=1.0 / D, bias=EPS)
            S.op("act", "activation", [ts_], [ts_], out=stat[:, 64 + si:65 + si], in_=stat[:, 32 + si:33 + si],
                 func=AF.Exp, scale=-0.5)
            S.op("dve", "tensor_scalar", [t_xt[i], ts_], [t_xnb[i]], out=xnb[i][:], in0=xt[i][:],
                 scalar1=stat[:, 64 + si:65 + si], scalar2=None, op0=ALU.mult)
            ps, tp = next_ps()
            psb = ps[:].bitcast(BF16)
            for k in range(8):
                S.op("pe", "transpose", [t_xnb[i], t_idb], [tp], out=psb[:, k * 128:(k + 1) * 128],
                     in_=xnb[i][:, k * 128:(k + 1) * 128], identity=ident_bf[:])
            S.op("dve", "tensor_tensor", [tp, t_g], [t_dst], out=dstT[:, :, dcol:dcol + 128],
                 in0=psb.rearrange("p (k n) -> p k n", k=8),
                 in1=gain[:, :].unsqueeze(2).to_broadcast([128, 8, 128]), op=ALU.mult)

        wmem = sb("wmem", [128, 8, 1024], BF16)
        t_wmem = [Trk() for _ in range(2)]
        for c, wsrc in enumerate((w_xk, w_xv)):
            for k in range(8):
                S.dma("pool", [], [t_wmem[c]], out=wmem[:, k, c * 512:(c + 1) * 512],
                      in_=wsrc[k * 128:(k + 1) * 128, :])

        wtm = sb("wtm", [128, 8, 1536], BF16)
        t_wtm = [Trk() for _ in range(3)]
        for c, c0 in enumerate((1024, 2560, 3072)):
            for k in range(8):
                S.dma("pool", [], [t_wtm[c]], out=wtm[:, k, c * 512:(c + 1) * 512],
                      in_=w_in[k * 128:(k + 1) * 128, c0:c0 + 512])
        t_ones = Trk()
        for tt in range(NTILE):
            S.op("pool", "memset", [], [t_vaug[tt]], vaug[:, tt, :, 128:132], 1.0)
        for tt in range(2):
            S.op("pool", "memset", [], [t_memvaug[tt]], memvaug[:, tt, :, 128:132], 1.0)

        kst = [sb("kst%d" % i, [128, 512], F32) for i in range(2)]
        t_kst = [Trk() for _ in range(2)]
        vst = [sb("vst%d" % i, [128, 512], F32) for i in range(2)]
        t_vst = [Trk() for _ in range(2)]
        kbf = [sb("kbf%d" % i, [128, 512], BF16) for i in range(2)]
        t_kbf = [Trk() for _ in range(2)]
        cnt["kv"] = 0

        def proj_kv(srcT, col0, t_src, w, wc_k, wc_v, t_wk, t_wv, k_dst, v_dst, kT_dst, kcol, t_kTd,
                    vaug_dst, t_vaugd):
            i = cnt["kv"] % 2
            cnt["kv"] += 1
            psk, tpk = next_ps()
            for k in range(8):
                S.op("pe", "matmul", [t_src, t_wk], [tpk], psk[:, :], lhsT=srcT[:, k, col0:col0 + 128],
                     rhs=w[:, k, wc_k:wc_k + 512], start=(k == 0), stop=(k == 7))
            psv, tpv = next_ps()
            for k in range(8):
                S.op("pe", "matmul", [t_src, t_wv], [tpv], psv[:, :], lhsT=srcT[:, k, col0:col0 + 128],
                     rhs=w[:, k, wc_v:wc_v + 512], start=(k == 0), stop=(k == 7))
            S.op("act", "activation", [tpk], [t_kst[i]], out=kst[i][:], in_=psk[:, :], func=AF.Copy)
            S.op("dve", "tensor_copy", [t_kst[i]], [t_kbf[i]], out=kbf[i][:], in_=kst[i][:])
            S.dma("pool", [t_kst[i]], [], out=k_dst, in_=kst[i][:])
            S.op("act", "activation", [tpv], [t_vst[i]], out=vst[i][:], in_=psv[:, :], func=AF.Copy)
            S.op("dve", "tensor_copy", [t_vst[i]], [t_vaugd], out=vaug_dst,
                 in_=vst[i][:].rearrange("p (h d) -> p h d", h=4))
            S.dma("pool", [t_vst[i]], [], out=v_dst, in_=vst[i][:])
            pst, tpt = next_ps()
            pstb = pst[:].bitcast(BF16)
            for h in range(4):
                S.op("pe", "transpose", [t_kbf[i], t_idb], [tpt], out=pstb[:, h * 128:(h + 1) * 128],
                     in_=kbf[i][:, h * 128:(h + 1) * 128], identity=ident_bf[:])
            S.op("act", "activation", [tpt], [t_kTd], out=kT_dst[:, :, kcol:kcol + 128],
                 in_=pstb[:, 0:512].rearrange("p (h n) -> p h n", h=4), func=AF.Copy)

        for m in range(2):
            norm_T(memp[m * 128:(m + 1) * 128, :], g_mem, memnT, m * 128, t_memnT[m])
            proj_kv(memnT, m * 128, t_memnT[m], wmem, 0, 512, t_wmem[0], t_wmem[1],
                    mk_o[m * 128:(m + 1) * 128, :], mv_o[m * 128:(m + 1) * 128, :],
                    memkT, m * 128, t_memkT[m], memvaug[:, m, :, 0:128], t_memvaug[m])

        norm_T(xall[0:128, :], g_mix, xnT, 0, t_xnT[0])
        for tt in range(NTILE):
            if tt + 1 < NTILE:
                norm_T(xall[(tt + 1) * 128:(tt + 2) * 128, :], g_mix, xnT, (tt + 1) * 128, t_xnT[tt + 1])
            proj_kv(xnT, tt * 128, t_xnT[tt], wtm, 512, 1024, t_wtm[1], t_wtm[2],
                    k_o[tt * 128:(tt + 1) * 128, :], v_o[tt * 128:(tt + 1) * 128, :],
                    kT, tt * 128, t_kT[tt], vaug[:, tt, :, 0:128], t_vaug[tt])
            psh, tph = next_ps()
            for k in range(8):
                S.op("pe", "matmul", [t_xnT[tt], t_wtm[0]], [tph], psh[:, :],
                     lhsT=xnT[:, k, tt * 128:(tt + 1) * 128], rhs=wtm[:, k, 0:512],
                     start=(k == 0), stop=(k == 7))
            S.op("act", "activation", [tph], [t_vhg[tt]], out=vhg[:, tt, :], in_=psh[:, :], func=AF.Copy)

        S.barrier()
        esB.close()
        esC = ExitStack()
        cur[0] = esC
        if STAGE >= 2:
            lraw = sb("lraw", [128, 8], F32)
            lb = sb("lb", [128, 16], F32)
            hgn = sb("hgn", [128, 4], F32)
            t_lb = Trk()
            with nc.allow_non_contiguous_dma(reason="tiny vectors"):
                S.dma("sp", [], [t_lb], out=lraw[:, :].rearrange("p (s h) -> p s h", s=2),
                      in_=lbl.rearrange("s (h p) -> p s h", p=128))
                S.dma("sp", [], [t_lb], out=hgn[:], in_=hgn_d.rearrange("o (h p) -> p (o h)", p=128))
            S.op("dve", "tensor_tensor", [t_lb], [t_lb], out=lb[:, 0:4], in0=lraw[:, 0:4], in1=lraw[:, 4:8],
                 op=ALU.subtract)
            S.op("act", "activation", [t_lb], [t_lb], out=lb[:, 4:8], in_=lb[:, 0:4], func=AF.Sigmoid)
            S.op("dve", "tensor_scalar", [t_lb], [t_lb], out=lb[:, 8:12], in0=lb[:, 4:8], scalar1=-1.0,
                 scalar2=1.0, op0=ALU.mult, op1=ALU.add)
            S.op("dve", "tensor_scalar", [t_lb], [t_lb], out=lb[:, 12:16], in0=lb[:, 8:12], scalar1=-1.0,
                 scalar2=None, op0=ALU.mult)
            mask64 = sb("mask64", [128, 128], F32)
            t_m = Trk()
            whg = sb("whg", [128, 8, 1536], BF16)
            t_whg = [Trk() for _ in range(3)]
            for c, c0 in enumerate((0, 512, 1536)):
                for k in range(8):
                    S.dma("pool", [], [t_whg[c]], out=whg[:, k, c * 512:(c + 1) * 512],
                          in_=w_in[k * 128:(k + 1) * 128, c0:c0 + 512])
            def wt(name, dt=F32, n=512):
                return sb(name, [128, n], dt), Trk()
            sig, t_sig = wt("h_sig")
            ff, t_ff = wt("h_f")
            bb, t_bb = wt("h_b")
            qs, t_qs = wt("h_qs")
            H4 = range(4)
            eb, t_eb = zip(*[wt("h_eb%d" % h) for h in H4])
            gs, t_gs = zip(*[wt("h_gs%d" % h, BF16) for h in H4])
            Qt, t_Qt = zip(*[wt("h_Qt%d" % h, BF16) for h in H4])
            Kt, t_Kt = zip(*[wt("h_Kt%d" % h, BF16) for h in H4])
            KhT, t_KhT = zip(*[wt("h_KhT%d" % h, BF16) for h in H4])
            Qt32, t_Qt32 = zip(*[wt("h_Qt32_%d" % h, F32, 128) for h in H4])
            Kh = [[sb("h_Kh%d_%d" % (h, i), [128, 128], BF16) for i in range(2)] for h in H4]
            t_Kh = [[Trk() for _ in range(2)] for _ in H4]
            sTm = [[sb("h_sTm%d_%d" % (h, i), [128, 128], BF16) for i in range(2)] for h in H4]
            t_sTm = [[Trk() for _ in range(2)] for _ in H4]
            Sf = [sb("h_S%d" % h, [128, 128], F32) for h in H4]
            Sb = [sb("h_Sb%d" % h, [128, 128], BF16) for h in H4]
            t_S = [Trk() for _ in H4]
            t_Sb = [Trk() for _ in H4]
            s0t = [sb("h_s0_%d" % i, [128, 128], F32) for i in range(8)]
            t_s0 = [Trk() for _ in range(8)]
            snew = [sb("h_sn_%d" % i, [128, 128], F32) for i in range(8)]
            t_sn = [Trk() for _ in range(8)]
            vm = sb("h_vm", [128, 8, 128], BF16)
            t_vm = Trk()
            cnt["s0"] = 0
            cnt["hg"] = 0

            def hg_ps():
                i = 4 + cnt["hg"] % 4
                cnt["hg"] += 1
                return PS[i], t_ps[i]

            for h in H4:
                S.op("dve", "memset", [], [t_S[h]], Sf[h][:], 0.0)
                S.op("pool", "memset", [], [t_Sb[h]], Sb[h][:], 0.0)
            for g in range(5):
                c0 = g * 512
                n = 512 if g < 4 else 128
                tsrc = [t_xnT[tt] for tt in range(g * 4, min(g * 4 + 4, NTILE))]
                C = 64 if g < 4 else TS
                for h in H4:
                    pss = []
                    for c in range(3):
                        ps, tp = hg_ps()
                        for k in range(8):
                            S.op("pe", "matmul", tsrc + [t_whg[c]], [tp], ps[:, 0:n],
                                 lhsT=whg[:, k, c * 512 + h * 128:c * 512 + (h + 1) * 128],
                                 rhs=xnT[:, k, c0:c0 + n], start=(k == 0), stop=(k == 7))
                        pss.append((ps, tp))
                    (psq, tpq), (psf, tpf), (psg, tpg) = pss
                    S.op("act", "activation", [tpf], [t_sig], out=sig[:, 0:n], in_=psf[:, 0:n], func=AF.Sigmoid)
                    S.op("act", "activation", [tpq], [t_qs], out=qs[:, 0:n], in_=psq[:, 0:n], func=AF.Silu)
                    S.op("act", "activation", [tpg], [t_gs[h]], out=gs[h][:, 0:n], in_=psg[:, 0:n], func=AF.Silu)
                    S.op("dve", "tensor_scalar", [t_sig, t_lb], [t_ff], out=ff[:, 0:n], in0=sig[:, 0:n],
                         scalar1=lb[:, 8 + h:9 + h], scalar2=lb[:, 4 + h:5 + h], op0=ALU.mult, op1=ALU.add)
                    S.op("dve", "tensor_scalar", [t_sig, t_lb], [t_sig], out=sig[:, 0:n], in0=sig[:, 0:n],
                         scalar1=lb[:, 12 + h:13 + h], scalar2=lb[:, 8 + h:9 + h], op0=ALU.mult, op1=ALU.add)
                    S.op("act", "activation", [t_ff], [t_ff], out=ff[:, 0:n], in_=ff[:, 0:n], func=AF.Ln)
                    rm = cst[:, 512:1024] if g < 4 else cst[:, 1024:1152]
                    S.op("dve", "tensor_tensor_scan", [t_ff, t_cst], [t_bb], out=bb[:, 0:n], data0=rm,
                         data1=ff[:, 0:n], initial=0.0, op0=ALU.mult, op1=ALU.add)
                    S.op("act", "activation", [t_bb], [t_eb[h]], out=eb[h][:, 0:n], in_=bb[:, 0:n], func=AF.Exp)
                    S.op("act", "activation", [t_bb], [t_ff], out=ff[:, 0:n], in_=bb[:, 0:n], func=AF.Exp,
                         scale=-1.0)
                    S.op("dve", "tensor_tensor", [t_qs, t_eb[h]], [t_Qt[h]], out=Qt[h][:, 0:n], in0=qs[:, 0:n],
                         in1=eb[h][:, 0:n], op=ALU.mult)
                    S.op("dve", "tensor_tensor", [t_sig, t_ff], [t_Kt[h]], out=Kt[h][:, 0:n], in0=sig[:, 0:n],
                         in1=ff[:, 0:n], op=ALU.mult)
                    for ci in range(n // C):
                        S.op("dve", "tensor_scalar", [t_Kt[h], t_eb[h]], [t_KhT[h]], out=KhT[h][:, ci * C:(ci + 1) * C],
                             in0=Kt[h][:, ci * C:(ci + 1) * C], scalar1=eb[h][:, (ci + 1) * C - 1:(ci + 1) * C],
                             scalar2=None, op0=ALU.mult)
                    if g == 4:
                        S.op("dve", "tensor_tensor", [t_qs, t_eb[h]], [t_Qt32[h]], out=Qt32[h][:, 0:128], in0=qs[:, 0:128],
                             in1=eb[h][:, 0:128], op=ALU.mult)
                pso = [PS[h] for h in H4]
                tpo = [t_ps[h] for h in H4]
                for ti in range(n // 128):
                    tt = g * 4 + ti
                    cs = ti * 128
                    ki = ti % 2
                    for h in H4:
                        pst, tpt = hg_ps()
                        pstb = pst[:].bitcast(BF16)
                        S.op("pe", "transpose", [t_KhT[h], t_idb], [tpt], out=pstb[:, 0:128],
                             in_=KhT[h][:, cs:cs + 128], identity=ident_bf[:])
                        S.op("act", "activation", [tpt], [t_Kh[h][ki]], out=Kh[h][ki][:], in_=pstb[:, 0:128], func=AF.Copy)
                        pss_, tps = hg_ps()
                        S.op("pe", "matmul", [t_Kt[h], t_Qt[h]], [tps], pss_[:, 0:128], lhsT=Kt[h][:, cs:cs + 128],
                             rhs=Qt[h][:, cs:cs + 128], start=True, stop=True)
                        mk_ = cst[:, 384:512] if g < 4 else cst[:, 256:384]
                        S.op("dve", "tensor_tensor", [tps, t_cst], [t_sTm[h][ki]], out=sTm[h][ki][:], in0=pss_[:, 0:128],
                             in1=mk_, op=ALU.mult)
                        S.op("pe", "matmul", [t_vhg[tt], t_sTm[h][ki]], [tpo[h]], pso[h][:, cs:cs + 128],
                             lhsT=vhg[:, tt, h * 128:(h + 1) * 128], rhs=sTm[h][ki][:], start=True, stop=False)
                    if g < 4:
                        for half in range(2):
                            r0 = half * 64
                            for h in H4:
                                S.op("pe", "matmul", [t_Sb[h], t_Qt[h]], [tpo[h]], pso[h][:, cs + r0:cs + r0 + 64], lhsT=Sb[h][:],
                                     rhs=Qt[h][:, cs + r0:cs + r0 + 64], start=False, stop=(half == 1))
                                psu, tpu = hg_ps()
                                S.op("pe", "matmul", [t_Kh[h][ki], t_vhg[tt]], [tpu], psu[:, 0:128],
                                     lhsT=Kh[h][ki][r0:r0 + 64, :], rhs=vhg[r0:r0 + 64, tt, h * 128:(h + 1) * 128],
                                     start=True, stop=True)
                                S.op("dve", "scalar_tensor_tensor", [t_S[h], t_eb[h], tpu], [t_S[h]], out=Sf[h][:], in0=Sf[h][:],
                                     scalar=eb[h][:, cs + r0 + 63:cs + r0 + 64], in1=psu[:, 0:128], op0=ALU.mult,
                                     op1=ALU.add)
                                S.op("act", "activation", [t_S[h]], [t_Sb[h]], out=Sb[h][:], in_=Sf[h][:], func=AF.Copy)
                    else:
                        for h in H4:
                            for jj in range(8):
                                S.op("dve", "tensor_scalar", [t_vhg[tt], t_cst], [t_vm], out=vm[:, jj, :],
                                     in0=vhg[:, tt, h * 128:(h + 1) * 128], scalar1=cst[:, 1280 + jj:1281 + jj],
                                     scalar2=None, op0=ALU.mult)
                            for i in range(NS):
                                bi = cnt["s0"] % 8
                                cnt["s0"] += 1
                                S.dma("sp", [], [t_s0[bi]], out=s0t[bi][:], in_=s0_d[i * 4 + h])
                                S.op("pe", "matmul", [t_s0[bi], t_Qt32[h]], [tpo[h]], pso[h][:, i * TS:(i + 1) * TS],
                                     lhsT=s0t[bi][:], rhs=Qt32[h][:, i * TS:(i + 1) * TS], start=False,
                                     stop=(i == NS - 1))
                                psu, tpu = hg_ps()
                                g32 = (i // 8) * 64
                                S.op("pe", "matmul", [t_Kh[h][ki], t_vm], [tpu], psu[:, 0:128],
                                     lhsT=Kh[h][ki][g32:g32 + 64, :], rhs=vm[g32:g32 + 64, i % 8, :],
                                     start=True, stop=True)
                                S.op("dve", "scalar_tensor_tensor", [t_s0[bi], t_eb[h], tpu], [t_sn[bi]],
                                     out=snew[bi][:], in0=s0t[bi][:], scalar=eb[h][:, i * TS + TS - 1:i * TS + TS],
                                     in1=psu[:, 0:128], op0=ALU.mult, op1=ALU.add)
                                S.dma("pool", [t_sn[bi]], [], out=ss_o[i * 4 + h], in_=snew[bi][:])
                if g == 3:
                    for h in H4:
                        S.dma("pool", [t_S[h]], [], out=sp_o[h], in_=Sf[h][:])
                for h in H4:
                    S.op("act", "activation", [tpo[h]], [t_sig], out=sig[:, 0:n], in_=pso[h][:, 0:n], func=AF.Square)
                    psm, tpm = hg_ps()
                    S.op("pe", "matmul", [t_sig, t_cst], [tpm], psm[:, 0:n], lhsT=cst[:, 1152:1280], rhs=sig[:, 0:n],
                         start=True, stop=True)
                    S.op("act", "activation", [tpm], [t_bb], out=bb[:, 0:n], in_=psm[:, 0:n], func=AF.Ln,
                         bias=EPS)
                    S.op("act", "activation", [t_bb], [t_bb], out=bb[:, 0:n], in_=bb[:, 0:n], func=AF.Exp,
                         scale=-0.5)
                    S.op("dve", "tensor_tensor", [tpo[h], t_bb], [t_ff], out=ff[:, 0:n], in0=pso[h][:, 0:n],
                         in1=bb[:, 0:n], op=ALU.mult)
                    S.op("dve", "scalar_tensor_tensor", [t_ff, t_lb, t_gs[h]], [t_oaT[h][g]], out=oaT[:, h, c0:c0 + n],
                         in0=ff[:, 0:n], scalar=hgn[:, h:h + 1], in1=gs[h][:, 0:n], op0=ALU.mult, op1=ALU.mult)

        S.barrier()
        esC.close()
        esA.close()
        cur[0] = esM

        esD = ExitStack()
        cur[0] = esD
        if STAGE >= 3:
            SCALE = float(128 ** -0.5)
            Eown = sb("Eown", [128, 4, 512], F32)
            Eadj = sb("Eadj", [128, 4, 512], F32)
            cfar = sb("cfar", [128, 4], F32)
            EownS = sb("EownS", [128, 4, 128], F32)
            esD0 = ExitStack()
            cur[0] = esD0
            relt = sb("relt", [32, 4], F32)
            oh = sb("oh", [32, 768], F32)
            crow = sb("crow", [4, 768], F32)
            et = sb("et", [4, 768], F32)
            t_rel, t_oh, t_et, t_scr, t_E = Trk(), Trk(), Trk(), Trk(), Trk()
            with nc.allow_non_contiguous_dma(reason="tiny table"):
                S.dma("sp", [], [t_rel], out=relt[:], in_=rel_d[:, :])
            S.dma("sp", [], [t_oh], out=oh[:], in_=consts2[0:32, :])
            S.dma("sp", [], [t_oh], out=crow[:], in_=consts2[32:36, :])
            for half in range(2):
                ps, tp = next_ps()
                S.op("pe", "matmul", [t_rel, t_oh], [tp], ps[0:4, 0:384], lhsT=relt[:, :],
                     rhs=oh[:, half * 384:(half + 1) * 384], start=True, stop=True)
                S.op("act", "activation", [tp], [t_et], out=et[:, half * 384:(half + 1) * 384], in_=ps[0:4, 0:384],
                     func=AF.Exp)
            S.op("dve", "tensor_tensor", [t_et, t_oh], [t_et], out=et[:], in0=et[:], in1=crow[:], op=ALU.mult)
            S.dma("sp", [t_et], [t_scr], out=et_scr[:, :], in_=et[:])
            hk = [sb("hk%d" % i, [128, 256], F32) for i in range(2)]
            t_hk = [Trk() for _ in range(2)]
            cnt["hk"] = 0
            with nc.allow_non_contiguous_dma(reason="toeplitz expansion of the bias vector"):
                for h in range(4):
                    for (Edst, cbase) in ((Eown, 256), (Eadj, 512)):
                        for j in range(2):
                            bi = cnt["hk"] % 2
                            cnt["hk"] += 1
                            S.dma("sp", [t_scr], [t_hk[bi]], out=hk[bi][:],
                                  in_=bass.AP(et_scr_t, h * 768 + cbase - 128 * j - 127, [[1, 128], [1, 256]]))
                            ps, tp = next_ps()
                            S.op("pe", "matmul", [t_hk[bi], t_cst], [tp], ps[:, 0:256], lhsT=cst[:, 1320:1448],
                                 rhs=hk[bi][:], start=True, stop=True)
                            S.op("act", "activation", [tp], [t_E], out=Edst[:, h, j * 256:(j + 1) * 256],
                                 in_=ps[:, 0:256], func=AF.Copy)
                    S.dma("sp", [t_scr], [t_E], out=cfar[:, h:h + 1],
                          in_=bass.AP(et_scr_t, h * 768 + 767, [[0, 128], [1, 1]]))
            for h in range(4):
                S.op("dve", "tensor_tensor", [t_E, t_cst], [t_E], out=EownS[:, h, :], in0=Eown[:, h, 0:128],
                     in1=cst[:, 256:384], op=ALU.mult)
            S.barrier()
            esD0.close()
            cur[0] = esD
            ptb = sb("ptb", [128, NS * 16], I32)
            idx = sb("idx", [128, NS * 16], I32)
            t_idx = Trk()
            with nc.allow_non_contiguous_dma(reason="page table broadcast"):
                S.dma("sp", [], [t_idx], out=ptb[:], in_=bass.AP(pt_d.tensor, 0, [[0, 128], [1, NS * 16]]))
            S.op("dve", "tensor_scalar", [t_idx, t_cst], [t_idx], out=idx[:], in0=ptb[:], scalar1=128.0,
                 scalar2=cst[:, 1300:1301], op0=ALU.mult, op1=ALU.add)
            wq = sb("wq", [128, 8, 512], BF16)
            t_wq = Trk()
            for k in range(8):
                S.dma("pool", [], [t_wq], out=wq[:, k, :], in_=w_in[k * 128:(k + 1) * 128, 2048:2560])
            qT = sb("qT", [128, NT], BF16)
            t_qT = [Trk() for _ in range(5)]
            qTs = sb("qTs", [128, 4, 128], BF16)
            pTo = sb("pTo", [128, 4, 128], BF16)
            t_qTs, t_pTo = Trk(), Trk()
            km32 = sb("km32", [128, 8], F32)
            kmb = sb("kmb", [128, 8], BF16)
            t_km = Trk()
            gsb = sb("gsb", [128, 8], F32)
            m8 = sb("m8", [128, 8], F32)
            selw = [sb("selw%d" % i, [128, 16], F32) for i in range(2)]
            t_sel = [Trk() for _ in range(2)]
            t_gs2 = Trk()
            acc = [sb("acc%d" % i, [128, 132], F32) for i in range(2)]
            t_acc = [Trk() for _ in range(2)]
            pT = [sb("pT%d" % i, [128, 512], BF16) for i in range(2)]
            t_pT = [Trk() for _ in range(2)]
            pTf = sb("pTf", [128, 512], F32)
            t_pTf = Trk()
            obt = sb("obt", [128, 128], BF16)
            rden = sb("rden", [128, 1], F32)
            t_obt = Trk()
            cnt["pT"] = 0
            alltk = list(t_kT[0:16])
            pTf2 = sb("pTf2", [128, 64], F32)
            t_pTf2 = Trk()

            def mb_ps():
                i = 3 + cnt["ps"] % 5
                cnt["ps"] += 1
                return PS[i], t_ps[i]
            next_ps = mb_ps
            for h in range(4):
                ps, tp = next_ps()
                for k in range(8):
                    S.op("pe", "matmul", [t_xnT[16], t_wq], [tp], ps[:, 0:128], lhsT=wq[:, k, h * 128:(h + 1) * 128],
                         rhs=xnT[:, k, 2048:2176], start=(k == 0), stop=(k == 7))
                S.op("act", "activation", [tp], [t_qTs], out=qTs[:, h, :], in_=ps[:, 0:128], func=AF.Copy)
                ps, tp = next_ps()
                S.op("pe", "matmul", [t_kT[16], t_qTs], [tp], ps[:, 0:128], lhsT=kT[:, h, 2048:2176],
                     rhs=qTs[:, h, :], start=True, stop=True)
                S.op("act", "activation", [tp], [t_pTf], out=pTf[:, 0:128], in_=ps[:, 0:128], func=AF.Exp, scale=SCALE)
                S.op("dve", "tensor_tensor", [t_pTf, t_E], [t_pTo], out=pTo[:, h, :], in0=pTf[:, 0:128],
                     in1=EownS[:, h, :], op=ALU.mult)
            kvpg = [sb("kvpg%d" % i, [128, 1024], F32) for i in range(3)]
            t_kvpg = [Trk() for _ in range(3)]
            kTs = sb("kTs", [128, 4, 2048], BF16)
            t_kTs = [Trk() for _ in range(16)]
            vaugs = sb("vaugs", [128, 16, 4, 132], BF16)
            t_vaugs = [Trk() for _ in range(16)]
            for pg in range(16):
                S.op("pool", "memset", [], [t_vaugs[pg]], vaugs[:, pg, :, 128:132], 1.0)
            kms32 = sb("kms32", [128, 32], F32)
            kmsb = sb("kmsb", [128, 32], BF16)
            t_kms = Trk()
            gss = sb("gss", [8, 32], F32)
            m8s = sb("m8s", [8, 8], F32)
            ws = sb("ws", [8, 64], F32)
            t_ws = Trk()
            pTs = sb("pTs", [128, 512], BF16)
            t_pTs = Trk()
            accs = sb("accs", [8, 4, 132], F32)
            t_accs = Trk()
            obs = sb("obs", [8, 4, 128], BF16)
            rdens = sb("rdens", [8, 4], F32)
            t_obs = Trk()
            def gen_sample():
                def gather(f):
                    S.dma("pool", [t_idx], [t_kvpg[f % 3]], meth="indirect_dma_start", out=kvpg[f % 3][:], out_offset=None,
                          in_=ckv_d, in_offset=bass.IndirectOffsetOnAxis(idx[:, f:f + 1], 0))
                gather(0)
                gather(1)
                for i in range(NS):
                    for pg in range(16):
                        f = i * 16 + pg
                        bi = f % 3
                        if f + 2 < NS * 16:
                            gather(f + 2)
                        ps, tp = next_ps()
                        for h in range(4):
                            S.op("pe", "transpose", [t_kvpg[bi], t_cst], [tp], out=ps[:, h * 128:(h + 1) * 128],
                                 in_=kvpg[bi][:, h * 128:(h + 1) * 128], identity=cst[:, 0:128])
                        S.op("act", "activation", [tp], [t_kTs[pg]], out=kTs[:, :, pg * 128:(pg + 1) * 128],
                             in_=ps[:, :].rearrange("p (h n) -> p h n", h=4), func=AF.Copy)
                        S.op("dve", "tensor_copy", [t_kvpg[bi]], [t_vaugs[pg]], out=vaugs[:, pg, :, 0:128],
                             in_=kvpg[bi][:, 512:1024].rearrange("p (h d) -> p h d", h=4))
                        yield
                    S.op("dve", "tensor_reduce", t_kTs, [t_kms], out=kms32[:, :],
                         in_=kTs[:, :, :].rearrange("p h (n k) -> p (h n) k", n=8), axis=AX.X, op=ALU.add)
                    S.op("dve", "tensor_copy", [t_kms], [t_kms], out=kmsb[:], in_=kms32[:])
                    ps, tp = next_ps()
                    for h in range(4):
                        S.op("pe", "matmul", [t_qTs, t_kms], [tp], ps[0:8, h * 8:(h + 1) * 8],
                             lhsT=qTs[:, h, i * TS:(i + 1) * TS], rhs=kmsb[:, h * 8:(h + 1) * 8], start=True, stop=True)
                    S.op("dve", "tensor_copy", [tp], [t_ws], out=gss[:], in_=ps[0:8, 0:32])
                    for h in range(4):
                        S.op("dve", "max", [t_ws], [t_ws], out=m8s[:], in_=gss[:, h * 8:(h + 1) * 8])
                        S.op("dve", "tensor_scalar", [t_ws], [t_ws], out=ws[:, h * 8:(h + 1) * 8], in0=gss[:, h * 8:(h + 1) * 8],
                             scalar1=m8s[:, 2:3], scalar2=None, op0=ALU.is_ge)
                        S.op("dve", "tensor_scalar", [t_ws, t_E], [t_ws], out=ws[:, 32 + h * 8:40 + h * 8],
                             in0=ws[:, h * 8:(h + 1) * 8], scalar1=cfar[0:8, h:h + 1], scalar2=None, op0=ALU.mult)
                    yield
                    pss, tps = PS[2], t_ps[2]
                    for pg in range(16):
                        for h in range(4):
                            cc = (pg * 4 + h) * TS
                            S.op("pe", "matmul", [t_kTs[pg], t_qTs], [tps], pss[:, cc:cc + TS],
                                 lhsT=kTs[:, h, pg * 128:(pg + 1) * 128], rhs=qTs[:, h, i * TS:(i + 1) * TS],
                                 start=True, stop=True)
                        if pg % 4 == 3:
                            yield
                    S.op("act", "activation", [tps], [t_pTs], out=pTs[:, 0:448], in_=pss[:, 0:448], func=AF.Exp, scale=SCALE)
                    S.op("act", "activation", [tps], [t_pTf2], out=pTf2[:, 0:64], in_=pss[:, 448:512], func=AF.Exp,
                         scale=SCALE)
                    S.op("dve", "tensor_tensor", [t_pTf2, t_E], [t_pTs],
                         out=pTs[:, 448:512].rearrange("p (j h q) -> p j h q", j=2, h=4),
                         in0=pTf2[:, 0:64].rearrange("p (j h q) -> p j h q", j=2, h=4),
                         in1=Eadj[:, :, :].rearrange("p h (j q) -> p j h q", j=2)[:, :, :, 0:TS], op=ALU.mult)
                    yield
                    for h in range(4):
                        for nb in range(8):
                            po, tpo2 = next_ps()
                            for j in range(2):
                                pg = 2 * nb + j
                                cc = (pg * 4 + h) * TS
                                S.op("pe", "matmul", [t_pTs, t_vaugs[pg]], [tpo2], po[0:8, 0:129], lhsT=pTs[:, cc:cc + TS],
                                     rhs=vaugs[:, pg, h, 0:129], start=(j == 0), stop=(j == 1))
                            w = ws[:, h * 8 + nb:h * 8 + nb + 1] if nb == 7 else ws[:, 32 + h * 8 + nb:33 + h * 8 + nb]
                            if nb == 0:
                                S.op("dve", "tensor_scalar", [tpo2, t_ws], [t_accs], out=accs[:, h, 0:129], in0=po[0:8, 0:129],
                                     scalar1=w, scalar2=None, op0=ALU.mult)
                            else:
                                S.op("dve", "scalar_tensor_tensor", [tpo2, t_ws, t_accs], [t_accs], out=accs[:, h, 0:129],
                                     in0=po[0:8, 0:129], scalar=w, in1=accs[:, h, 0:129], op0=ALU.mult, op1=ALU.add)
                        po, tpo2 = next_ps()
                        S.op("pe", "matmul", [t_pTo, t_vaug[16]], [tpo2], po[0:8, 0:129], lhsT=pTo[:, h, i * TS:(i + 1) * TS],
                             rhs=vaug[:, 16, h, 0:129], start=True, stop=True)
                        S.op("dve", "tensor_tensor", [tpo2, t_accs], [t_accs], out=accs[:, h, 0:129], in0=po[0:8, 0:129],
                             in1=accs[:, h, 0:129], op=ALU.add)
                        yield
                    S.op("dve", "reciprocal", [t_accs], [t_obs], out=rdens[:, :], in_=accs[:, :, 128])
                    S.op("dve", "tensor_tensor", [t_accs, t_obs], [t_obs], out=obs[:, :, :], in0=accs[:, :, 0:128],
                         in1=rdens[:, :].unsqueeze(2).to_broadcast([8, 4, 128]), op=ALU.mult)
                    ps, tp = next_ps()
                    psb = ps[:].bitcast(BF16)
                    for h in range(4):
                        S.op("pe", "transpose", [t_obs, t_idb], [tp], out=psb[:, h * TS:(h + 1) * TS], in_=obs[:, h, :],
                             identity=ident_bf[0:8, 0:8])
                    S.op("act", "activation", [tp], [t_obT[16]], out=obT[:, :, 2048 + i * TS:2048 + (i + 1) * TS],
                         in_=psb[:, 0:4 * TS].rearrange("p (h q) -> p h q", h=4), func=AF.Copy)
                    yield
            SGEN = gen_sample()

            def adv(k):
                for _ in range(k):
                    next(SGEN, None)

            for h in range(4):
                for g in range(4):
                    c0 = g * 512
                    n = 512
                    tsrc = [t_xnT[tt] for tt in range(g * 4, g * 4 + 4)]
                    ps, tp = next_ps()
                    for k in range(8):
                        S.op("pe", "matmul", tsrc + [t_wq], [tp], ps[:, 0:n], lhsT=wq[:, k, h * 128:(h + 1) * 128],
                             rhs=xnT[:, k, c0:c0 + n], start=(k == 0), stop=(k == 7))
                    S.op("act", "activation", [tp], [t_qT[g]], out=qT[:, c0:c0 + n], in_=ps[:, 0:n], func=AF.Copy)
                S.op("dve", "tensor_reduce", alltk, [t_km], out=km32[:, :],
                     in_=kT[:, h, 0:2048].rearrange("p (n k) -> p n k", n=8), axis=AX.X, op=ALU.add)
                S.op("dve", "tensor_copy", [t_km], [t_km], out=kmb[:], in_=km32[:])
                units = [(b, nb) for b in range(8) for nb in range(b + 1)]

                def QK(b, nb):
                    tq = t_qT[b // 2]
                    ps, tp = long_ps()
                    for j in range(2):
                        kc = nb * 256 + j * 128
                        S.op("pe", "matmul", [t_kT[2 * nb + j], tq], [tp], ps[:, j * 256:(j + 1) * 256],
                             lhsT=kT[:, h, kc:kc + 128], rhs=qT[:, b * 256:(b + 1) * 256], start=True, stop=True)
                    return ps, tp

                def GATE(b):
                    tq = t_qT[b // 2]
                    for qi in range(2):
                        ps, tp = next_ps()
                        qc = b * 256 + qi * 128
                        S.op("pe", "matmul", [tq, t_km], [tp], ps[:, 0:8], lhsT=qT[:, qc:qc + 128], rhs=kmb[:, :],
                             start=True, stop=True)
                        S.op("dve", "memset", [], [t_gs2], gsb[:], -1.0e30)
                        S.op("dve", "tensor_copy", [tp, t_gs2], [t_gs2], out=gsb[:, 0:b], in_=ps[:, 0:b])
                        S.op("dve", "max", [t_gs2], [t_gs2], out=m8[:], in_=gsb[:])
                        S.op("dve", "tensor_scalar", [t_gs2], [t_sel[qi]], out=selw[qi][:, 0:8], in0=gsb[:],
                             scalar1=m8[:, 2:3], scalar2=None, op0=ALU.is_ge)
                        S.op("dve", "tensor_scalar", [t_sel[qi], t_E], [t_sel[qi]], out=selw[qi][:, 8:16],
                             in0=selw[qi][:, 0:8], scalar1=cfar[:, h:h + 1], scalar2=None, op0=ALU.mult)

                def EXPPV(b, nb, ps, tp):
                    pi = cnt["pT"] % 2
                    cnt["pT"] += 1
                    if nb <= b - 2:
                        S.op("act", "activation", [tp], [t_pT[pi]], out=pT[pi][:], in_=ps[:, :], func=AF.Exp,
                             scale=SCALE)
                    else:
                        S.op("act", "activation", [tp], [t_pTf], out=pTf[:], in_=ps[:, :], func=AF.Exp, scale=SCALE)
                        Et = Eown if nb == b else Eadj
                        S.op("dve", "tensor_tensor", [t_pTf, t_E], [t_pT[pi]], out=pT[pi][:], in0=pTf[:],
                             in1=Et[:, h, :], op=ALU.mult)
                    po, tpo2 = next_ps()
                    for qi in range(2):
                        for j in range(2):
                            S.op("pe", "matmul", [t_pT[pi], t_vaug[2 * nb + j]], [tpo2],
                                 po[:, qi * 256:qi * 256 + 129],
                                 lhsT=pT[pi][:, j * 256 + qi * 128:j * 256 + qi * 128 + 128],
                                 rhs=vaug[:, 2 * nb + j, h, 0:129], start=(j == 0), stop=(j == 1))
                    for qi in range(2):
                        if nb == b:
                            w, rw = 1.0, []
                        elif nb == b - 1:
                            w, rw = (selw[qi][:, nb:nb + 1], [t_sel[qi]]) if b >= 4 else (1.0, [])
                        else:
                            w, rw = (selw[qi][:, 8 + nb:9 + nb], [t_sel[qi]]) if b >= 4 else (cfar[:, h:h + 1], [t_E])
                        src_ = po[:, qi * 256:qi * 256 + 129]
                        if nb == 0:
                            S.op("dve", "tensor_scalar", [tpo2] + rw, [t_acc[qi]], out=acc[qi][:, 0:129], in0=src_,
                                 scalar1=w, scalar2=None, op0=ALU.mult)
                        else:
                            S.op("dve", "scalar_tensor_tensor", [tpo2, t_acc[qi]] + rw, [t_acc[qi]],
                                 out=acc[qi][:, 0:129], in0=src_, scalar=w, in1=acc[qi][:, 0:129], op0=ALU.mult,
                                 op1=ALU.add)

                def FINAL(b):
                    for qi in range(2):
                        tt = 2 * b + qi
                        S.op("dve", "reciprocal", [t_acc[qi]], [t_obt], out=rden[:], in_=acc[qi][:, 128:129])
                        S.op("dve", "tensor_scalar", [t_acc[qi], t_obt], [t_obt], out=obt[:], in0=acc[qi][:, 0:128],
                             scalar1=rden[:, 0:1], scalar2=None, op0=ALU.mult)
                        ps, tp = next_ps()
                        psb = ps[:].bitcast(BF16)
                        S.op("pe", "transpose", [t_obt, t_idb], [tp], out=psb[:, 0:128], in_=obt[:], identity=ident_bf[:])
                        S.op("act", "activation", [tp], [t_obT[tt]], out=obT[:, h, tt * 128:(tt + 1) * 128],
                             in_=psb[:, 0:128], func=AF.Copy)

                pend = QK(*units[0])
                for u, (b, nb) in enumerate(units):
                    cur_qk = pend
                    if u + 1 < len(units):
                        pend = QK(*units[u + 1])
                    if nb == 0 and b >= 4:
                        GATE(b)
                    EXPPV(b, nb, *cur_qk)
                    if nb == b:
                        FINAL(b)
                    adv(3)

            for _ in SGEN:
                pass
            if DEBUG:
                S.dma("sp", t_obT, [], out=dbg_o[:, :], in_=obT[:, :, :].rearrange("p h n -> p (h n)"))
        S.barrier()
        esD.close()
        cur[0] = esM

        esE = ExitStack()
        cur[0] = esE
        t_mrgG = [Trk() for _ in range(5)]
        if STAGE >= 4:
            wg = sb("wg", [128, 8, 2048], BF16)
            t_wg = [Trk() for _ in range(4)]
            for c in range(4):
                for k in range(8):
                    S.dma("pool", [], [t_wg[c]], out=wg[:, k, c * 512:(c + 1) * 512],
                          in_=w_in[k * 128:(k + 1) * 128, 3584 + c * 512:3584 + (c + 1) * 512])
            wab = sb("wab", [128, 2, 4, 1024], BF16)
            t_wab = Trk()
            for c, wsrc in enumerate((wa_d, wb_d)):
                for k in range(4):
                    for hf in range(2):
                        S.dma("pool", [], [t_wab], out=wab[:, c, k, hf * 512:(hf + 1) * 512],
                              in_=wsrc[k * 128:(k + 1) * 128, hf * 512:(hf + 1) * 512])
            sga = sb("sga", [128, 512], F32)
            sgb = sb("sgb", [128, 512], F32)
            m1 = sb("m1", [128, 512], F32)
            m2 = sb("m2", [128, 512], F32)
            t_sga, t_sgb, t_m1, t_m2 = Trk(), Trk(), Trk(), Trk()
            gunits = [(j, g) for j in range(8) for g in range(5)]

            def GMM(u):
                j, g = gunits[u]
                c0 = g * 512
                n = 512 if g < 4 else 128
                tiles = list(range(g * 4, min(g * 4 + 4, NTILE)))
                tx = [t_xnT[tt] for tt in tiles]
                base = 4 * (u % 2)
                (psga, tpga), (psgb, tpgb), (psa, tpa), (psb_, tpb) = [(PS[base + q], t_ps[base + q]) for q in range(4)]
                for k in range(8):
                    S.op("pe", "matmul", tx + [t_wg[j // 4]], [tpga], psga[:, 0:n], lhsT=wg[:, k, j * 128:(j + 1) * 128],
                         rhs=xnT[:, k, c0:c0 + n], start=(k == 0), stop=(k == 7))
                for k in range(8):
                    S.op("pe", "matmul", tx + [t_wg[2 + j // 4]], [tpgb], psgb[:, 0:n],
                         lhsT=wg[:, k, 1024 + j * 128:1024 + (j + 1) * 128], rhs=xnT[:, k, c0:c0 + n],
                         start=(k == 0), stop=(k == 7))
                for k in range(4):
                    S.op("pe", "matmul", [t_oaT[k][g], t_wab], [tpa], psa[:, 0:n], lhsT=wab[:, 0, k, j * 128:(j + 1) * 128],
                         rhs=oaT[:, k, c0:c0 + n], start=(k == 0), stop=(k == 3))
                for k in range(4):
                    S.op("pe", "matmul", [t_obT[tt] for tt in tiles] + [t_wab], [tpb], psb_[:, 0:n],
                         lhsT=wab[:, 1, k, j * 128:(j + 1) * 128], rhs=obT[:, k, c0:c0 + n], start=(k == 0), stop=(k == 3))
                return (psga, tpga), (psgb, tpgb), (psa, tpa), (psb_, tpb)

            def GELEM(u, banks):
                j, g = gunits[u]
                c0 = g * 512
                n = 512 if g < 4 else 128
                (psga, tpga), (psgb, tpgb), (psa, tpa), (psb_, tpb) = banks
                S.op("act", "activation", [tpga], [t_sga], out=sga[:, 0:n], in_=psga[:, 0:n], func=AF.Sigmoid)
                S.op("act", "activation", [tpgb], [t_sgb], out=sgb[:, 0:n], in_=psgb[:, 0:n], func=AF.Sigmoid)
                S.op("dve", "tensor_tensor", [tpa, t_sga], [t_m1], out=m1[:, 0:n], in0=psa[:, 0:n], in1=sga[:, 0:n], op=ALU.mult)
                S.op("dve", "tensor_tensor", [tpb, t_sgb], [t_m2], out=m2[:, 0:n], in0=psb_[:, 0:n], in1=sgb[:, 0:n], op=ALU.mult)
                S.op("dve", "tensor_tensor", [t_m1, t_m2], [t_mrgG[g]], out=mergedT[:, j, c0:c0 + n], in0=m1[:, 0:n],
                     in1=m2[:, 0:n], op=ALU.add)

            gb_prev = GMM(0)
            for u in range(len(gunits)):
                gb_next = GMM(u + 1) if u + 1 < len(gunits) else None
                GELEM(u, gb_prev)
                gb_prev = gb_next
        S.barrier()
        esE.close()
        esM.close()
        cur[0] = es

        xres = sb("xres", [128, NTILE, D], F32)
        t_xr = [Trk() for _ in range(NTILE)]
        comb = sb("comb", [128, NTILE, 32], F32)
        t_comb = [Trk() for _ in range(NTILE)]
        junk2 = sb("junk2", [128, D], BF16)
        t_junk2 = Trk()
        xnb2 = [sb("xnb2_%d" % i, [128, D], BF16) for i in range(2)]
        t_xnb2 = [Trk() for _ in range(2)]
        gvec = sb("gvec", [128, 16], F32)
        t_gv = Trk()
        with nc.allow_non_contiguous_dma(reason="tiny gain vectors"):
            S.dma("sp", [], [t_gv], out=gvec[:, 0:8], in_=norm_xa_d.rearrange("o (k p) -> p (o k)", p=128))
            S.dma("sp", [], [t_gv], out=gvec[:, 8:16], in_=norm_ffn_d.rearrange("o (k p) -> p (o k)", p=128))

        wxq = sb("wxq", [128, 8, 512], BF16)
        wxo = sb("wxo", [128, 4, D], BF16)
        t_wxq, t_wxo = Trk(), Trk()
        wgu = [sb("wgu%d" % i, [128, 8, 512], BF16) for i in range(2)]
        wd = [sb("wd%d" % i, [128, 2, D], BF16) for i in range(2)]
        t_wgu = [Trk() for _ in range(2)]
        t_wd = [Trk() for _ in range(2)]

        stg, t_stg = [], []
        cnt["stg"] = 0

        def load_expert(e):
            bi = e % 2
            for piece in range(3):
                r = cnt["stg"] % 2
                cnt["stg"] += 1
                if piece < 2:
                    wsrc = (weg_d, weu_d)[piece]
                    S.dma("sp", [], [t_stg[r]], out=stg[r][:, :].rearrange("p (k n) -> p k n", k=8),
                          in_=wsrc[e].rearrange("(k p) n -> p k n", p=128))
                    S.op("pool", "tensor_copy", [t_stg[r]], [t_wgu[bi]], out=wgu[bi][:, :, piece * 256:(piece + 1) * 256],
                         in_=stg[r][:, :].rearrange("p (k n) -> p k n", k=8))
                else:
                    S.dma("sp", [], [t_stg[r]], out=stg[r][:, :].rearrange("p (k n) -> p k n", k=2),
                          in_=wed_d[e].rearrange("(k p) n -> p k n", p=128))
                    S.op("pool", "tensor_copy", [t_stg[r]], [t_wd[bi]], out=wd[bi][:, :, :],
                         in_=stg[r][:, :].rearrange("p (k n) -> p k n", k=2))

        def norm_S(tt, gain, dstT, dcol, t_dst):
            i = cnt["x"] % 2
            cnt["x"] += 1
            si = cnt["st"] % 32
            cnt["st"] += 1
            ts_ = t_stat[si]
            S.op("act", "activation", [t_xr[tt]], [t_junk2, ts_], out=junk2[:], in_=xres[:, tt, :], func=AF.Square,
                 accum_out=stat[:, si:si + 1])
            S.op("act", "activation", [ts_], [ts_], out=stat[:, 32 + si:33 + si], in_=stat[:, si:si + 1],
                 func=AF.Ln, scale=1.0 / D, bias=EPS)
            S.op("act", "activation", [ts_], [ts_], out=stat[:, 64 + si:65 + si], in_=stat[:, 32 + si:33 + si],
                 func=AF.Exp, scale=-0.5)
            S.op("dve", "tensor_scalar", [t_xr[tt], ts_], [t_xnb2[i]], out=xnb2[i][:], in0=xres[:, tt, :],
                 scalar1=stat[:, 64 + si:65 + si], scalar2=None, op0=ALU.mult)
            ps, tp = next_ps()
            psb = ps[:].bitcast(BF16)
            for k in range(8):
                S.op("pe", "transpose", [t_xnb2[i], t_idb], [tp], out=psb[:, k * 128:(k + 1) * 128],
                     in_=xnb2[i][:, k * 128:(k + 1) * 128], identity=ident_bf[:])
            S.op("dve", "tensor_tensor", [tp, t_gv], [t_dst], out=dstT[:, :, dcol:dcol + 128],
                 in0=psb.rearrange("p (k n) -> p k n", k=8),
                 in1=gain.unsqueeze(2).to_broadcast([128, 8, 128]), op=ALU.mult)
            return si

        esF = ExitStack()
        cur[0] = esF
        if STAGE >= 4:
            wmix = sb("wmix", [128, 8, D], BF16)
            t_wmix = Trk()
            for k in range(8):
                for hf in range(2):
                    S.dma("pool", [], [t_wmix], out=wmix[:, k, hf * 512:(hf + 1) * 512],
                          in_=wmix_d[k * 128:(k + 1) * 128, hf * 512:(hf + 1) * 512])
            for k in range(8):
                S.dma("pool", [], [t_wxq], out=wxq[:, k, :], in_=wxq_d[k * 128:(k + 1) * 128, :])
            for k in range(4):
                for hf in range(2):
                    S.dma("pool", [], [t_wxo], out=wxo[:, k, hf * 512:(hf + 1) * 512],
                          in_=wxo_d[k * 128:(k + 1) * 128, hf * 512:(hf + 1) * 512])
            for tt in range(NTILE):
                S.dma("sp", [], [t_xr[tt]], out=xres[:, tt, :], in_=xall[tt * 128:(tt + 1) * 128, :])
                for hf in range(2):
                    ps, tp = next_ps()
                    for k in range(8):
                        S.op("pe", "matmul", [t_mrgG[tt // 4], t_wmix], [tp], ps[:, :], lhsT=mergedT[:, k, tt * 128:(tt + 1) * 128],
                             rhs=wmix[:, k, hf * 512:(hf + 1) * 512], start=(k == 0), stop=(k == 7))
                    S.op("dve", "tensor_tensor", [tp, t_xr[tt]], [t_xr[tt]], out=xres[:, tt, hf * 512:(hf + 1) * 512],
                         in0=ps[:, :], in1=xres[:, tt, hf * 512:(hf + 1) * 512], op=ALU.add)
        S.barrier()
        esF.close()

        esG = ExitStack()
        cur[0] = esG
        if STAGE >= 5:
            SCALE = float(128 ** -0.5)
            x1nT = sb("x1nT", [128, 8, 512], BF16)
            t_x1n = [Trk() for _ in range(4)]
            xqT = sb("xqT", [128, 4, 512], BF16)
            t_xq = Trk()
            pTx = [sb("pTx%d" % j, [128, 512], BF16) for j in range(2)]
            t_pTx = [Trk() for _ in range(2)]
            xa = sb("xa", [128, 4, 4, 128], BF16)
            t_xa = [Trk() for _ in range(4)]
            rdx = sb("rdx", [128, 4], F32)
            t_rdx = Trk()
            xaT = sb("xaT", [128, 4, 512], BF16)
            t_xaT = [Trk() for _ in range(4)]
            mst = [sb("mst%d" % i, [128, 512], F32) for i in range(4)]
            t_mst = [Trk() for _ in range(4)]
            mkTs = sb("mkTs", [128, 4, 256], BF16)
            t_mkTs = Trk()
            mvs = sb("mvs", [128, 2, 4, 132], BF16)
            t_mvs = Trk()
            S.op("pool", "memset", [], [t_mvs], mvs[:, :, :, 128:132], 1.0)
            pTxs = sb("pTxs", [128, 64], BF16)
            t_pTxs = Trk()
            xas = sb("xas", [8, 4, 128], BF16)
            rdxs = sb("rdxs", [8, 4], F32)
            t_xas = Trk()
            for g in range(5):
                n = 512 if g < 4 else 128
                tiles = list(range(g * 4, min(g * 4 + 4, NTILE)))
                for ti, tt in enumerate(tiles):
                    norm_S(tt, gvec[:, 0:8], x1nT, ti * 128, t_x1n[ti])
                tx = [t_x1n[ti] for ti in range(len(tiles))]
                for h in range(4):
                    ps, tp = next_ps()
                    for k in range(8):
                        S.op("pe", "matmul", tx + [t_wxq], [tp], ps[:, 0:n], lhsT=wxq[:, k, h * 128:(h + 1) * 128],
                             rhs=x1nT[:, k, 0:n], start=(k == 0), stop=(k == 7))
                    S.op("act", "activation", [tp], [t_xq], out=xqT[:, h, 0:n], in_=ps[:, 0:n], func=AF.Copy)
                if g < 4:
                    for h in range(4):
                        for j in range(2):
                            ps, tp = next_ps()
                            S.op("pe", "matmul", [t_memkT[j], t_xq], [tp], ps[:, 0:n], lhsT=memkT[:, h, j * 128:(j + 1) * 128],
                                 rhs=xqT[:, h, 0:n], start=True, stop=True)
                            S.op("act", "activation", [tp], [t_pTx[j]], out=pTx[j][:, 0:n], in_=ps[:, 0:n], func=AF.Exp,
                                 scale=SCALE)
                        for ti in range(4):
                            po, tpo2 = next_ps()
                            for j in range(2):
                                S.op("pe", "matmul", [t_pTx[j], t_memvaug[j]], [tpo2], po[:, 0:129],
                                     lhsT=pTx[j][:, ti * 128:(ti + 1) * 128], rhs=memvaug[:, j, h, 0:129],
                                     start=(j == 0), stop=(j == 1))
                            S.op("dve", "reciprocal", [tpo2], [t_rdx], out=rdx[:, 0:1], in_=po[:, 128:129])
                            S.op("dve", "tensor_scalar", [tpo2, t_rdx], [t_xa[ti]], out=xa[:, ti, h, :], in0=po[:, 0:128],
                                 scalar1=rdx[:, 0:1], scalar2=None, op0=ALU.mult)
                    for ti in range(4):
                        ps, tp = next_ps()
                        psb = ps[:].bitcast(BF16)
                        for h in range(4):
                            S.op("pe", "transpose", [t_xa[ti], t_idb], [tp], out=psb[:, h * 128:(h + 1) * 128],
                                 in_=xa[:, ti, h, :], identity=ident_bf[:])
                        S.op("act", "activation", [tp], [t_xaT[ti]], out=xaT[:, :, ti * 128:(ti + 1) * 128],
                             in_=psb[:, 0:512].rearrange("p (h n) -> p h n", h=4), func=AF.Copy)
                else:
                    def mload(i):
                        o = 0
                        for j in range(2):
                            S.dma("sp", [], [t_mst[o + j]], out=mst[o + j][:], in_=cmk_d[i * 256 + j * 128:i * 256 + (j + 1) * 128, :])
                            S.dma("sp", [], [t_mst[o + 2 + j]], out=mst[o + 2 + j][:],
                                  in_=cmv_d[i * 256 + j * 128:i * 256 + (j + 1) * 128, :])
                    for i in range(NS):
                        mload(i)
                        o = 0
                        for j in range(2):
                            ps, tp = next_ps()
                            for h in range(4):
                                S.op("pe", "transpose", [t_mst[o + j], t_cst], [tp], out=ps[:, h * 128:(h + 1) * 128],
                                     in_=mst[o + j][:, h * 128:(h + 1) * 128], identity=cst[:, 0:128])
                            S.op("act", "activation", [tp], [t_mkTs], out=mkTs[:, :, j * 128:(j + 1) * 128],
                                 in_=ps[:, :].rearrange("p (h n) -> p h n", h=4), func=AF.Copy)
                            S.op("dve", "tensor_copy", [t_mst[o + 2 + j]], [t_mvs], out=mvs[:, j, :, 0:128],
                                 in_=mst[o + 2 + j][:, :].rearrange("p (h d) -> p h d", h=4))
                        ps, tp = next_ps()
                        for j in range(2):
                            for h in range(4):
                                cc = (j * 4 + h) * TS
                                S.op("pe", "matmul", [t_mkTs, t_xq], [tp], ps[:, cc:cc + TS], lhsT=mkTs[:, h, j * 128:(j + 1) * 128],
                                     rhs=xqT[:, h, i * TS:(i + 1) * TS], start=True, stop=True)
                        S.op("act", "activation", [tp], [t_pTxs], out=pTxs[:, :], in_=ps[:, 0:64], func=AF.Exp, scale=SCALE)
                        for h in range(4):
                            po, tpo2 = next_ps()
                            for j in range(2):
                                cc = (j * 4 + h) * TS
                                S.op("pe", "matmul", [t_pTxs, t_mvs], [tpo2], po[0:8, 0:129], lhsT=pTxs[:, cc:cc + TS],
                                     rhs=mvs[:, j, h, 0:129], start=(j == 0), stop=(j == 1))
                            S.op("dve", "reciprocal", [tpo2], [t_xas], out=rdxs[:, h:h + 1], in_=po[0:8, 128:129])
                            S.op("dve", "tensor_scalar", [tpo2, t_xas], [t_xas], out=xas[:, h, :], in0=po[0:8, 0:128],
                                 scalar1=rdxs[:, h:h + 1], scalar2=None, op0=ALU.mult)
                        ps, tp = next_ps()
                        psb = ps[:].bitcast(BF16)
                        for h in range(4):
                            S.op("pe", "transpose", [t_xas, t_idb], [tp], out=psb[:, h * TS:(h + 1) * TS], in_=xas[:, h, :],
                                 identity=ident_bf[0:8, 0:8])
                        S.op("act", "activation", [tp], [t_xaT[0]], out=xaT[:, :, i * TS:(i + 1) * TS],
                             in_=psb[:, 0:4 * TS].rearrange("p (h q) -> p h q", h=4), func=AF.Copy)
                for ti, tt in enumerate(tiles):
                    for hf in range(2):
                        ps, tp = next_ps()
                        for h in range(4):
                            S.op("pe", "matmul", [t_xaT[ti], t_wxo], [tp], ps[:, :], lhsT=xaT[:, h, ti * 128:(ti + 1) * 128],
                                 rhs=wxo[:, h, hf * 512:(hf + 1) * 512], start=(h == 0), stop=(h == 3))
                        S.op("dve", "tensor_tensor", [tp, t_xr[tt]], [t_xr[tt]], out=xres[:, tt, hf * 512:(hf + 1) * 512],
                             in0=ps[:, :], in1=xres[:, tt, hf * 512:(hf + 1) * 512], op=ALU.add)
        S.barrier()
        esG.close()

        esH = ExitStack()
        cur[0] = esH
        xfT = mergedT
        t_xf = [Trk() for _ in range(NTILE)]
        if STAGE >= 6:
            wr = sb("wr", [128, 8, 36], BF16)
            t_wr = Trk()
            with nc.allow_non_contiguous_dma(reason="narrow router weights"):
                for k in range(8):
                    S.dma("pool", [], [t_wr], out=wr[:, k, 0:4], in_=wrg_d[k * 128:(k + 1) * 128, :])
                    S.dma("pool", [], [t_wr], out=wr[:, k, 4:36], in_=wre_d[k * 128:(k + 1) * 128, :])
            rb = sb("rb", [128, 36], F32)
            t_rb = Trk()
            with nc.allow_non_contiguous_dma(reason="bias broadcast"):
                S.dma("sp", [], [t_rb], out=rb[:, 0:4], in_=bass.AP(brg_d.tensor, 0, [[0, 128], [1, 4]]))
                S.dma("sp", [], [t_rb], out=rb[:, 4:36], in_=bass.AP(bre_d.tensor, 0, [[0, 128], [1, 32]]))
            lg = sb("lg", [128, 36], F32)
            rt = sb("rt", [128, 64], F32)
            t_rt = Trk()
            for tt in range(NTILE):
                norm_S(tt, gvec[:, 8:16], xfT, tt * 128, t_xf[tt])
                ps, tp = next_ps()
                for k in range(8):
                    S.op("pe", "matmul", [t_xf[tt], t_wr], [tp], ps[:, 0:36], lhsT=xfT[:, k, tt * 128:(tt + 1) * 128],
                         rhs=wr[:, k, :], start=(k == 0), stop=(k == 7))
                R_, W_ = [t_rt], [t_rt]
                S.op("dve", "tensor_tensor", [tp, t_rb], W_, out=lg[:], in0=ps[:, 0:36], in1=rb[:], op=ALU.add)
                S.op("dve", "tensor_reduce", R_, W_, out=rt[:, 0:1], in_=lg[:, 0:4], axis=AX.X, op=ALU.max)
                S.op("dve", "tensor_scalar", R_, W_, out=rt[:, 1:2], in0=rt[:, 0:1], scalar1=-1.0, scalar2=None, op0=ALU.mult)
                S.op("dve", "tensor_scalar", R_, W_, out=rt[:, 4:8], in0=lg[:, 0:4], scalar1=rt[:, 0:1], scalar2=None,
                     op0=ALU.is_equal)
                S.op("act", "activation", R_, W_, out=rt[:, 8:12], in_=lg[:, 0:4], func=AF.Exp, bias=rt[:, 1:2],
                     accum_out=rt[:, 2:3])
                S.op("dve", "reciprocal", R_, W_, out=rt[:, 3:4], in_=rt[:, 2:3])
                S.op("dve", "tensor_scalar", R_, W_, out=rt[:, 16:24], in0=lg[:, 4:12], scalar1=rt[:, 4:5], scalar2=None,
                     op0=ALU.mult)
                for gi in range(1, 4):
                    S.op("dve", "scalar_tensor_tensor", R_, W_, out=rt[:, 16:24], in0=lg[:, 4 + gi * 8:12 + gi * 8],
                         scalar=rt[:, 4 + gi:5 + gi], in1=rt[:, 16:24], op0=ALU.mult, op1=ALU.add)
                S.op("dve", "max", R_, W_, out=rt[:, 24:32], in_=rt[:, 16:24])
                S.op("dve", "tensor_tensor", R_, W_, out=rt[:, 32:33], in0=rt[:, 25:26], in1=rt[:, 24:25], op=ALU.subtract)
                S.op("act", "activation", R_, W_, out=rt[:, 33:34], in_=rt[:, 32:33], func=AF.Sigmoid)
                S.op("dve", "tensor_scalar", R_, W_, out=rt[:, 34:35], in0=rt[:, 33:34], scalar1=-1.0, scalar2=1.0,
                     op0=ALU.mult, op1=ALU.add)
                S.op("dve", "tensor_scalar", R_, W_, out=rt[:, 34:35], in0=rt[:, 34:35], scalar1=rt[:, 3:4], scalar2=None,
                     op0=ALU.mult)
                S.op("dve", "tensor_scalar", R_, W_, out=rt[:, 33:34], in0=rt[:, 33:34], scalar1=rt[:, 3:4], scalar2=None,
                     op0=ALU.mult)
                S.op("dve", "tensor_scalar", R_, W_, out=rt[:, 40:48], in0=rt[:, 16:24], scalar1=rt[:, 24:25],
                     scalar2=rt[:, 34:35], op0=ALU.is_equal, op1=ALU.mult)
                S.op("dve", "tensor_scalar", R_, W_, out=rt[:, 48:56], in0=rt[:, 16:24], scalar1=rt[:, 25:26],
                     scalar2=rt[:, 33:34], op0=ALU.is_equal, op1=ALU.mult)
                S.op("dve", "tensor_tensor", R_, W_, out=rt[:, 40:48], in0=rt[:, 40:48], in1=rt[:, 48:56], op=ALU.add)
                for gi in range(4):
                    S.op("dve", "tensor_scalar", R_, [t_comb[tt]], out=comb[:, tt, gi * 8:(gi + 1) * 8], in0=rt[:, 40:48],
                         scalar1=rt[:, 4 + gi:5 + gi], scalar2=None, op0=ALU.mult)
            sgl = [sb("sgl%d" % i, [128, 512], F32) for i in range(2)]
            t_sgl = [Trk() for _ in range(2)]
            hT = [sb("hT%d" % i, [128, 2, 512], BF16) for i in range(2)]
            t_hT = [[Trk() for _ in range(2)] for _ in range(2)]
            cnt["gu"] = 0
            cnt["dn"] = 0

            def gu_ps():
                i = 2 + cnt["gu"] % 4
                cnt["gu"] += 1
                return PS[i], t_ps[i]

            def dn_ps():
                i = (0, 1, 6, 7)[cnt["dn"] % 4]
                cnt["dn"] += 1
                return PS[i], t_ps[i]

            def geom(g):
                return g * 512, (512 if g < 4 else 128), list(range(g * 4, min(g * 4 + 4, NTILE)))

            def GU(e, g):
                bi = e % 2
                c0, n, tiles = geom(g)
                tx = [t_xf[tt] for tt in tiles]
                pgu = []
                for c in range(4):
                    ps, tp = gu_ps()
                    for k in range(8):
                        S.op("pe", "matmul", tx + [t_wgu[bi]], [tp], ps[:, 0:n], lhsT=wgu[bi][:, k, c * 128:(c + 1) * 128],
                             rhs=xfT[:, k, c0:c0 + n], start=(k == 0), stop=(k == 7))
                    pgu.append((ps, tp))
                return pgu

            def ELEM(step, g, pgu):
                c0, n, tiles = geom(g)
                hb = step % 2
                for fh in range(2):
                    S.op("act", "activation", [pgu[fh][1]], [t_sgl[fh]], out=sgl[fh][:, 0:n], in_=pgu[fh][0][:, 0:n],
                         func=AF.Silu)
                    S.op("dve", "tensor_tensor", [pgu[2 + fh][1], t_sgl[fh]], [t_hT[hb][fh]], out=hT[hb][:, fh, 0:n],
                         in0=pgu[2 + fh][0][:, 0:n], in1=sgl[fh][:, 0:n], op=ALU.mult)

            def DOWN(step, e, g):
                bi = e % 2
                hb = step % 2
                c0, n, tiles = geom(g)
                for ti, tt in enumerate(tiles):
                    for hf in range(2):
                        ps, tp = dn_ps()
                        for fh in range(2):
                            S.op("pe", "matmul", [t_hT[hb][fh], t_wd[bi]], [tp], ps[:, :], lhsT=hT[hb][:, fh, ti * 128:(ti + 1) * 128],
                                 rhs=wd[bi][:, fh, hf * 512:(hf + 1) * 512], start=(fh == 0), stop=(fh == 1))
                        S.op("dve", "scalar_tensor_tensor", [tp, t_comb[tt], t_xr[tt]], [t_xr[tt]],
                             out=xres[:, tt, hf * 512:(hf + 1) * 512], in0=ps[:, :], scalar=comb[:, tt, e:e + 1],
                             in1=xres[:, tt, hf * 512:(hf + 1) * 512], op0=ALU.mult, op1=ALU.add)

            for i_ in range(2):
                stg.append(sb("stg%d" % i_, [128, 2048], F32))
                t_stg.append(Trk())
            load_expert(0)
            steps = [(e, g) for e in range(32) for g in range(5)]
            prev = None
            for si_, (e, g) in enumerate(steps):
                pgu = GU(e, g)
                if prev is not None:
                    DOWN(si_ - 1, *prev)
                if g == 0 and e + 1 < 32:
                    load_expert(e + 1)
                ELEM(si_, g, pgu)
                prev = (e, g)
            DOWN(len(steps) - 1, *prev)
        gfin = sb("gfin", [128, D], F32)
        t_gfin = Trk()
        with nc.allow_non_contiguous_dma(reason="gain broadcast"):
            S.dma("sp", [], [t_gfin], out=gfin[:], in_=bass.AP(nfin_d.tensor, 0, [[0, 128], [1, D]]))
        yt = [sb("yt%d" % i, [128, D], F32) for i in range(2)]
        t_yt = [Trk() for _ in range(2)]
        for tt in range(NTILE):
            si = cnt["st"] % 32
            cnt["st"] += 1
            ts_ = t_stat[si]
            S.op("act", "activation", [t_xr[tt]], [t_junk2, ts_], out=junk2[:], in_=xres[:, tt, :], func=AF.Square,
                 accum_out=stat[:, si:si + 1])
            S.op("act", "activation", [ts_], [ts_], out=stat[:, 32 + si:33 + si], in_=stat[:, si:si + 1],
                 func=AF.Ln, scale=1.0 / D, bias=EPS)
            S.op("act", "activation", [ts_], [ts_], out=stat[:, 64 + si:65 + si], in_=stat[:, 32 + si:33 + si],
                 func=AF.Exp, scale=-0.5)
            yi = tt % 2
            S.op("dve", "scalar_tensor_tensor", [t_xr[tt], ts_, t_gfin], [t_yt[yi]], out=yt[yi][:], in0=xres[:, tt, :],
                 scalar=stat[:, 64 + si:65 + si], in1=gfin[:], op0=ALU.mult, op1=ALU.mult)
            S.dma("sp", [t_yt[yi]], [], out=y_o[tt * 128:(tt + 1) * 128, :], in_=yt[yi][:])
        S.barrier()
        esH.close()
        cur[0] = es

        S.finish()
    return nc


_CACHE = {}


def make_ckv(I):
    npool = I["cache_k"].shape[1]
    return np.concatenate([np.asarray(I["cache_k"])[0].reshape(npool * 128, 512),
                           np.asarray(I["cache_v"])[0].reshape(npool * 128, 512)], axis=1)


def make_in_map(c, I):
    f = lambda a: np.ascontiguousarray(np.asarray(a))
    xall = np.concatenate([f(I["x_prompt"])[c], f(I["x_sample"])[c * NS:(c + 1) * NS].reshape(NS * TS, D)], axis=0)
    npool = I["cache_k"].shape[1]
    return {
        "xall": xall,
        "memp": f(I["mem_prompt"])[c],
        "consts": host_consts(),
        "consts2": host_consts2(),
        "norm_mix": f(I["norm_mix"]), "norm_mem": f(I["norm_mem"]),
        "w_in": f(I["w_in"])[0], "w_xk": f(I["w_xk"])[0], "w_xv": f(I["w_xv"])[0],
        "lbl": f(I["hg_lb_logits"]), "hg_norm": f(I["hg_norm"]),
        "s0": f(I["state_hgrn"])[0, c * NS:(c + 1) * NS].reshape(NS * 4, 128, 128),
        "rel_table": f(I["rel_table"]),
        "cache_kv": I["_ckv"],
        "page_table": f(I["page_table"])[c * NS:(c + 1) * NS],
        "w_branch_a": f(I["w_branch_a"])[0], "w_branch_b": f(I["w_branch_b"])[0], "w_mix_out": f(I["w_mix_out"])[0],
        "norm_xattn": f(I["norm_xattn"]), "w_xq": f(I["w_xq"])[0], "w_xo": f(I["w_xo"])[0],
        "cache_mem_k": f(I["cache_mem_k"])[0, c * NS:(c + 1) * NS].reshape(NS * 256, 512),
        "cache_mem_v": f(I["cache_mem_v"])[0, c * NS:(c + 1) * NS].reshape(NS * 256, 512),
        "norm_ffn": f(I["norm_ffn"]), "w_group_router": f(I["w_group_router"])[0],
        "w_expert_router": f(I["w_expert_router"])[0], "b_group_router": f(I["b_group_router"]),
        "b_expert_router": f(I["b_expert_router"]),
        "w_expert_gate": f(I["w_expert_gate"])[0].reshape(32, D, 256),
        "w_expert_up": f(I["w_expert_up"])[0].reshape(32, D, 256),
        "w_expert_down": f(I["w_expert_down"])[0].reshape(32, 256, D),
        "norm_final": f(I["norm_final"]).reshape(1, D),
    }


def kernel(**I):
    npool = I["cache_k"].shape[1]
    key = ("nc", npool)
    if key not in _CACHE:
        _CACHE[key] = build_nc(npool)
    nc = _CACHE[key]
    I = dict(I)
    I["_ckv"] = make_ckv(I)
    in_maps = [make_in_map(c, I) for c in range(NCORES)]
    res = run_bass_kernel_spmd(nc, in_maps, core_ids=list(range(NCORES)))
    R = res.results
    cat = lambda name, sl: np.stack([R[c][name][sl] for c in range(NCORES)])
    y_prompt = cat("y", slice(0, T))
    y_sample = cat("y", slice(T, NT)).reshape(128, TS, D)
    k_prompt = cat("kout", slice(0, T)).reshape(1, 8, T, 4, 128)
    v_prompt = cat("vout", slice(0, T)).reshape(1, 8, T, 4, 128)
    k_sample = cat("kout", slice(T, NT)).reshape(1, 128, TS, 4, 128)
    v_sample = cat("vout", slice(T, NT)).reshape(1, 128, TS, 4, 128)
    sp = cat("spout", slice(None)).reshape(1, 8, 4, 128, 128)
    ss = cat("ssout", slice(None)).reshape(1, 128, 4, 128, 128)
    mk = cat("mkout", slice(None)).reshape(1, 8, 256, 4, 128)
    mv = cat("mvout", slice(None)).reshape(1, 8, 256, 4, 128)
    return (y_prompt, y_sample, k_prompt, v_prompt, sp, mk, mv, k_sample, v_sample, ss)
```
